# Optimizing a Trainium2 kernel written in Bass

```python
import jax, jax.numpy as jnp
from jax import lax
import numpy as np

D_MODEL = 1024
BATCH = 4
SEQ = 4096
DEPTH = 2

HEAD_DIM = 64
ROPE_THETA = 10000.0
DIL_CONFIGS = ((128, 1), (512, 4), (2048, 16))
N_DIL_GROUPS = 3
DIL_HEADS = 4
DIL_WIDTH = N_DIL_GROUPS * DIL_HEADS * HEAD_DIM
DIL_OUT = DIL_HEADS * HEAD_DIM
SB_HEADS = 8
SB_WIDTH = SB_HEADS * HEAD_DIM
SB_BLOCK = 128
POOL_WINDOWS = (2, 4, 8, 16)
POOL_GROUP = 128
POOL_WIDTH = len(POOL_WINDOWS) * POOL_GROUP
N_BRANCH = 3
IN_WIDTH = 3 * DIL_WIDTH + 3 * SB_WIDTH + POOL_WIDTH
MOE_GROUPS = 4
EXP_PER_GROUP = 4
N_EXPERTS = MOE_GROUPS * EXP_PER_GROUP
TOP_K_INNER = 2
EXP_HIDDEN = 256
PLE_DIM = 256
EPS = 1e-6

kernel_name = "hybrid_dilated_stickbreak_pool_hmoe"


def rmsnorm(x, g):
    xf = x.astype(jnp.float32)
    y = xf * lax.rsqrt(jnp.mean(xf * xf, axis=-1, keepdims=True) + EPS)
    return (y * g.astype(jnp.float32)).astype(x.dtype)


def rope(t, positions):
    half = t.shape[-1] // 2
    inv = ROPE_THETA ** (-jnp.arange(half, dtype=jnp.float32) / half)
    ang = positions.astype(jnp.float32)[..., None] * inv
    cos = jnp.cos(ang)[:, :, None, :]
    sin = jnp.sin(ang)[:, :, None, :]
    t1 = t[..., :half].astype(jnp.float32)
    t2 = t[..., half:].astype(jnp.float32)
    return jnp.concatenate([t1 * cos - t2 * sin, t2 * cos + t1 * sin], axis=-1).astype(t.dtype)


def dilated_attention(q, k, v, window, dilation):
    b, s, h, c = q.shape
    n = window // dilation
    L = s // dilation
    nb = -(-L // n)
    pad = nb * n - L

    def to_blocks(t):
        t = t.reshape(b, L, dilation, h, c).transpose(0, 2, 1, 3, 4)
        t = jnp.pad(t, ((0, 0), (0, 0), (0, pad), (0, 0), (0, 0)))
        return t.reshape(b, dilation, nb, n, h, c)

    def with_prev(t):
        prev = jnp.pad(t[:, :, :-1], ((0, 0), (0, 0), (1, 0), (0, 0), (0, 0), (0, 0)))
        return jnp.concatenate([prev, t], axis=3)

    qb = to_blocks(q)
    kk = with_prev(to_blocks(k))
    vv = with_prev(to_blocks(v))
    scores = jnp.einsum('brnqhc,brnkhc->brnhqk', qb, kk,
                        preferred_element_type=jnp.float32) * (c ** -0.5)
    qi = jnp.arange(n)[:, None]
    kj = jnp.arange(2 * n)[None, :]
    band = (kj >= qi) & (kj <= qi + n)
    has_prev = (jnp.arange(nb) > 0)[:, None, None]
    mask = band[None] & (has_prev | (kj >= n)[None])
    scores = jnp.where(mask[:, None], scores, -jnp.inf)
    m = jnp.max(scores, axis=-1, keepdims=True)
    e = jnp.exp(scores - m)
    den = jnp.sum(e, axis=-1, keepdims=True)
    lse = (m + jnp.log(den))[..., 0]
    o = jnp.einsum('brnhqk,brnkhc->brnqhc', (e / den).astype(v.dtype), vv)
    o = o.reshape(b, dilation, nb * n, h, c)[:, :, :L].transpose(0, 2, 1, 3, 4).reshape(b, s, h, c)
    lse = lse.transpose(0, 1, 2, 4, 3).reshape(b, dilation, nb * n, h)[:, :, :L]
    lse = lse.transpose(0, 2, 1, 3).reshape(b, s, h)
    return o, lse


def dilated_mixture(q, k, v):
    outs, lses = [], []
    for g, (w, d) in enumerate(DIL_CONFIGS):
        o, l = dilated_attention(q[:, :, g], k[:, :, g], v[:, :, g], w, d)
        outs.append(o.astype(jnp.float32))
        lses.append(l)
    wts = jax.nn.softmax(jnp.stack(lses, axis=0), axis=0)
    o = jnp.sum(wts[..., None] * jnp.stack(outs, axis=0), axis=0)
    return o.astype(q.dtype)


def stick_breaking_attention(q, k, v):
    b, s, h, c = q.shape
    nb = s // SB_BLOCK
    kpos = jnp.arange(s)

    def block(ib):
        start = ib * SB_BLOCK
        qb = lax.dynamic_slice_in_dim(q, start, SB_BLOCK, axis=1)
        z = jnp.einsum('bqhc,bkhc->bhqk', qb, k,
                       preferred_element_type=jnp.float32) * (c ** -0.5)
        qpos = start + jnp.arange(SB_BLOCK)
        mask = kpos[None, :] < qpos[:, None]
        log_om = jnp.where(mask, jax.nn.log_sigmoid(-z), 0.0)
        after = lax.cumsum(log_om, axis=3, reverse=True) - log_om
        a = jnp.where(mask, jnp.exp(jax.nn.log_sigmoid(z) + after), 0.0)
        return jnp.einsum('bhqk,bkhc->bqhc', a.astype(v.dtype), v)

    o = lax.map(block, jnp.arange(nb))
    return o.transpose(1, 0, 2, 3, 4).reshape(b, s, h, c)


def pool_mixer(u, w_pool, pool_scale):
    b, s, _ = u.shape
    ng = len(POOL_WINDOWS)
    uf = u.astype(jnp.float32).reshape(b, s, ng, POOL_GROUP)
    cs = jnp.concatenate([jnp.zeros((b, 1, ng, POOL_GROUP), jnp.float32),
                          jnp.cumsum(uf, axis=1)], axis=1)
    pos = jnp.arange(s)
    outs = []
    for g, w in enumerate(POOL_WINDOWS):
        csg = cs[:, :, g]
        lower = jnp.pad(csg, ((0, 0), (w - 1, 0), (0, 0)))[:, :s]
        upper = csg[:, 1:]
        cnt = jnp.minimum(pos + 1, w).astype(jnp.float32)[None, :, None]
        outs.append((upper - lower) / cnt - uf[:, :, g])
    pooled = jnp.stack(outs, axis=2).astype(u.dtype)
    y = jnp.einsum('bsgc,gcd->bsgd', pooled, w_pool).reshape(b, s, POOL_WIDTH)
    return y * pool_scale


def hierarchical_moe(h, w_rg, b_rg, w_re, b_re, w_g, w_u, w_d):
    b, s, d = h.shape
    t = h.reshape(b * s, d)
    lg = jnp.einsum('td,dg->tg', t, w_rg, preferred_element_type=jnp.float32) + b_rg
    pg = jax.nn.softmax(lg, axis=-1)
    p1, gi = lax.top_k(pg, 1)
    le = jnp.einsum('td,gde->tge', t, w_re, preferred_element_type=jnp.float32) + b_re
    le_sel = jnp.take_along_axis(le, gi[:, :, None], axis=1)[:, 0]
    v2, ei = lax.top_k(le_sel, TOP_K_INNER)
    w2 = jax.nn.softmax(v2, axis=-1) * p1
    eid = gi * EXP_PER_GROUP + ei
    gate = jnp.sum(jax.nn.one_hot(eid, N_EXPERTS, dtype=jnp.float32) * w2[..., None], axis=1)
    hid = jax.nn.silu(jnp.einsum('td,edf->tef', t, w_g)) * jnp.einsum('td,edf->tef', t, w_u)
    y = jnp.einsum('tef,efd->td', hid * gate[:, :, None].astype(hid.dtype), w_d)
    return y.reshape(b, s, d)


def setup_inputs(seed: int = 0) -> dict:
    key = jax.random.key(seed)
    ks = jax.random.split(key, 26)
    f32 = jnp.float32
    nrm = lambda k, shape, scale: jax.random.normal(k, shape, f32) * scale
    L, D = DEPTH, D_MODEL
    return {
        "x": nrm(ks[0], (BATCH, SEQ, D), 1.0),
        "p": nrm(ks[1], (DEPTH, BATCH, SEQ, PLE_DIM), 1.0),
        "positions": jnp.broadcast_to(jnp.arange(SEQ, dtype=jnp.int32)[None, :], (BATCH, SEQ)),
        "norm_mix": 1.0 + nrm(ks[2], (L, D), 0.05),
        "w_in": nrm(ks[3], (L, D, IN_WIDTH), D ** -0.5),
        "w_gate": nrm(ks[4], (L, D, N_BRANCH * D), D ** -0.5),
        "b_gate": nrm(ks[5], (L, N_BRANCH * D), 0.1),
        "w_pool": nrm(ks[6], (L, len(POOL_WINDOWS), POOL_GROUP, POOL_GROUP), POOL_GROUP ** -0.5),
        "pool_scale": 1.0 + nrm(ks[7], (L, POOL_WIDTH), 0.1),
        "w_up_a": nrm(ks[8], (L, DIL_OUT, D), DIL_OUT ** -0.5),
        "w_up_b": nrm(ks[9], (L, SB_WIDTH, D), SB_WIDTH ** -0.5),
        "w_up_c": nrm(ks[10], (L, POOL_WIDTH, D), POOL_WIDTH ** -0.5),
        "w_out": nrm(ks[11], (L, D, D), D ** -0.5),
        "norm_moe": 1.0 + nrm(ks[12], (L, D), 0.05),
        "w_router_grp": nrm(ks[13], (L, D, MOE_GROUPS), D ** -0.5),
        "b_router_grp": nrm(ks[14], (L, MOE_GROUPS), 0.01),
        "w_router_exp": nrm(ks[15], (L, MOE_GROUPS, D, EXP_PER_GROUP), D ** -0.5),
        "b_router_exp": nrm(ks[16], (L, MOE_GROUPS, EXP_PER_GROUP), 0.01),
        "w_exp_gate": nrm(ks[17], (L, N_EXPERTS, D, EXP_HIDDEN), D ** -0.5),
        "w_exp_up": nrm(ks[18], (L, N_EXPERTS, D, EXP_HIDDEN), D ** -0.5),
        "w_exp_down": nrm(ks[19], (L, N_EXPERTS, EXP_HIDDEN, D), EXP_HIDDEN ** -0.5),
        "norm_ple": 1.0 + nrm(ks[20], (L, D), 0.05),
        "w_ple_in": nrm(ks[21], (L, PLE_DIM, D), PLE_DIM ** -0.5),
        "w_ple_gate": nrm(ks[22], (L, D, D), D ** -0.5),
        "norm_final": 1.0 + nrm(ks[23], (D,), 0.05),
    }


def reference(x, p, positions, norm_mix, w_in, w_gate, b_gate, w_pool, pool_scale,
              w_up_a, w_up_b, w_up_c, w_out, norm_moe, w_router_grp, b_router_grp,
              w_router_exp, b_router_exp, w_exp_gate, w_exp_up, w_exp_down,
              norm_ple, w_ple_in, w_ple_gate, norm_final):
    b, s, d = x.shape
    splits = [DIL_WIDTH, 2 * DIL_WIDTH, 3 * DIL_WIDTH, 3 * DIL_WIDTH + SB_WIDTH,
              3 * DIL_WIDTH + 2 * SB_WIDTH, 3 * DIL_WIDTH + 3 * SB_WIDTH]
    nd = N_DIL_GROUPS * DIL_HEADS
    for i in range(DEPTH):
        h = rmsnorm(x, norm_mix[i])
        proj = h @ w_in[i]
        qa, ka, va, qb, kb, vb, uc = jnp.split(proj, splits, axis=-1)
        qa = rope(qa.reshape(b, s, nd, HEAD_DIM), positions).reshape(b, s, N_DIL_GROUPS, DIL_HEADS, HEAD_DIM)
        ka = rope(ka.reshape(b, s, nd, HEAD_DIM), positions).reshape(b, s, N_DIL_GROUPS, DIL_HEADS, HEAD_DIM)
        va = va.reshape(b, s, N_DIL_GROUPS, DIL_HEADS, HEAD_DIM)
        ya = dilated_mixture(qa, ka, va).reshape(b, s, DIL_OUT) @ w_up_a[i]
        yb = stick_breaking_attention(qb.reshape(b, s, SB_HEADS, HEAD_DIM),
                                      kb.reshape(b, s, SB_HEADS, HEAD_DIM),
                                      vb.reshape(b, s, SB_HEADS, HEAD_DIM)).reshape(b, s, SB_WIDTH) @ w_up_b[i]
        yc = pool_mixer(uc, w_pool[i], pool_scale[i]) @ w_up_c[i]
        gates = jax.nn.sigmoid(h @ w_gate[i] + b_gate[i]).reshape(b, s, N_BRANCH, d)
        merged = gates[:, :, 0] * ya + gates[:, :, 1] * yb + gates[:, :, 2] * yc
        x = x + merged @ w_out[i]
        x = x + hierarchical_moe(rmsnorm(x, norm_moe[i]), w_router_grp[i], b_router_grp[i],
                                 w_router_exp[i], b_router_exp[i], w_exp_gate[i],
                                 w_exp_up[i], w_exp_down[i])
        ple_gate = jax.nn.sigmoid(rmsnorm(x, norm_ple[i]) @ w_ple_gate[i])
        x = x + (p[i] @ w_ple_in[i]) * ple_gate
    return rmsnorm(x, norm_final)
```

```python
import contextlib
import numpy as np
import concourse.bass as bass
import concourse.mybir as mybir
from concourse.bass_utils import run_bass_kernel_spmd

F32, BF16, I32 = mybir.dt.float32, mybir.dt.bfloat16, mybir.dt.int32
AF = mybir.ActivationFunctionType
ALU = mybir.AluOpType
AX = mybir.AxisListType

D = 1024
S = 4096
DEPTH = 2
NKC = 8
TT = 512
NTT = S // TT
INW = 4352
DILW = 768
SBW = 512
Q_D, K_D, V_D = 0, 768, 1536
Q_S, K_S, V_S = 2304, 2816, 3328
U_C = 3840
DIL = ((128, 1), (512, 4), (2048, 16))
POOLW = (2, 4, 8, 16)
NEXP = 16
EH = 256
PLE = 256
EPS = 1e-6
TS = 2048
SAME_SYNC = True
PLE_PIPE = True
EXPERT_PIPE = False


class DSem:
    def __init__(self, sem):
        self.sem = sem
        self.cnt = 0


class Buf:
    __slots__ = ("name", "w", "r", "ds")

    def __init__(self, name):
        self.name = name
        self.w = {}
        self.r = {}
        self.ds = None


class Prog:
    def __init__(self, nc):
        self.nc = nc
        self.eng = {"pe": nc.tensor, "act": nc.scalar, "dve": nc.vector, "pool": nc.gpsimd, "sp": nc.sync}
        self.sem = {k: nc.alloc_semaphore("cs_" + k) for k in self.eng}
        self.cnt = {k: 0 for k in self.eng}
        self.seen = {k: {} for k in self.eng}
        self.dsems = []
        self.ds_by_id = {}
        self.free_ds = []
        self.n_ins = 0

    def _get_ds(self):
        if self.free_ds:
            return self.free_ds.pop()
        ds = DSem(self.nc.alloc_semaphore("ds_%d" % len(self.dsems)))
        self.dsems.append(ds)
        self.ds_by_id[id(ds.sem)] = ds
        return ds

    def buf(self, name, dma=False):
        b = Buf(name)
        if dma:
            b.ds = self._get_ds()
        return b

    def release(self, bufs):
        for b in bufs:
            if b.ds is not None:
                self.free_ds.append(b.ds)
                b.ds = None

    def _wait_tok(self, e, k, sem, val):
        ds = self.ds_by_id.get(k)
        if ds is not None:
            val = max(val, ds.cnt)
        seen = self.seen[e]
        if seen.get(k, 0) < val:
            self.eng[e].wait_ge(sem, val)
            seen[k] = val

    def _deps(self, e, r, w):
        own = id(self.sem[e])
        for b in r:
            for k, (sem, val) in b.w.items():
                if k == own and (e == "pe" or not SAME_SYNC):
                    continue
                self._wait_tok(e, k, sem, val)
        for b in w:
            for k, (sem, val) in b.w.items():
                if k == own:
                    continue
                self._wait_tok(e, k, sem, val)
            for k, (sem, val) in b.r.items():
                if k == own:
                    continue
                self._wait_tok(e, k, sem, val)

    def op(self, e, fn, r=(), w=()):
        self._deps(e, r, w)
        ins = fn()
        self.n_ins += 1
        self.cnt[e] += 1
        sem = self.sem[e]
        ins.then_inc(sem, 1)
        tok = (sem, self.cnt[e])
        k = id(sem)
        for b in r:
            b.r[k] = tok
        for b in w:
            b.w[k] = tok
        return ins

    def dma(self, q, out, in_, owner, r=(), w=(), slow=False):
        self._deps(q, r, w)
        if slow:
            ins = self.eng[q].dma_start(out=out, in_=in_, allow_slow_non_contiguous=True)
        else:
            ins = self.eng[q].dma_start(out=out, in_=in_)
        self.n_ins += 1
        ds = owner.ds
        ds.cnt += 16
        ins.then_inc(ds.sem, 16)
        tok = (ds.sem, ds.cnt)
        k = id(ds.sem)
        for b in r:
            b.r[k] = tok
        for b in w:
            b.w[k] = tok
        return ins

    def barrier(self):
        for e in self.eng:
            for e2 in self.eng:
                if e2 != e and self.cnt[e2] > 0:
                    self._wait_tok(e, id(self.sem[e2]), self.sem[e2], self.cnt[e2])
            for ds in self.dsems:
                if ds.cnt > 0:
                    self._wait_tok(e, id(ds.sem), ds.sem, ds.cnt)


class Ctx:
    pass


def act(P, out, in_, func, r, w, bias=None, scale=None, accum_out=None):
    kw = {}
    if bias is not None:
        kw["bias"] = bias
    if scale is not None:
        kw["scale"] = scale
    if accum_out is not None:
        kw["accum_out"] = accum_out
    return P.op("act", lambda: P.nc.scalar.activation(out=out, in_=in_, func=func, **kw), r=r, w=w)


def veng(P, e):
    return P.nc.vector if e == "dve" else P.nc.gpsimd


def tt(P, e, out, in0, in1, op, r, w):
    return P.op(e, lambda: veng(P, e).tensor_tensor(out=out, in0=in0, in1=in1, op=op), r=r, w=w)


def ts(P, e, out, in0, s1, op0, r, w, s2=None, op1=None):
    if op1 is None:
        return P.op(e, lambda: veng(P, e).tensor_scalar(out=out, in0=in0, scalar1=s1, scalar2=None, op0=op0), r=r, w=w)
    return P.op(e, lambda: veng(P, e).tensor_scalar(out=out, in0=in0, scalar1=s1, scalar2=s2, op0=op0, op1=op1), r=r, w=w)


def stt(P, e, out, in0, scalar, in1, op0, op1, r, w):
    return P.op(e, lambda: veng(P, e).scalar_tensor_tensor(out=out, in0=in0, scalar=scalar, in1=in1, op0=op0, op1=op1), r=r, w=w)


def cp(P, e, out, in_, r, w):
    if e == "act":
        return P.op("act", lambda: P.nc.scalar.copy(out=out, in_=in_), r=r, w=w)
    return P.op(e, lambda: veng(P, e).tensor_copy(out=out, in_=in_), r=r, w=w)


def mm(P, out, lhsT, rhs, start, stop, r, w):
    return P.op("pe", lambda: P.nc.tensor.matmul(out, lhsT=lhsT, rhs=rhs, start=start, stop=stop), r=r, w=w)


def tr(P, out, in_, ident, r, w):
    return P.op("pe", lambda: P.nc.tensor.transpose(out, in_, ident), r=r, w=w)


def norm_T(P, C, xblk, xb, gbT, outT, outT_b, col0, junk, junk_b, ss, ss_b, xs, xs_b, tps, tps_b,
           outF=None, outF_b=None, gb_b=None):
    nc = P.nc
    gb_b = gb_b if gb_b is not None else C.gb_b
    act(P, junk[:], xblk, AF.Square, r=[xb], w=[junk_b, ss_b], accum_out=ss[:, 0:1])
    act(P, ss[:, 1:2], ss[:, 0:1], AF.Ln, r=[ss_b, C.cst_b], w=[ss_b], scale=1.0 / D, bias=C.eps_col[:, 0:1])
    act(P, ss[:, 2:3], ss[:, 1:2], AF.Exp, r=[ss_b], w=[ss_b], scale=-0.5)
    ts(P, "dve", xs[:], xblk, ss[:, 2:3], ALU.mult, r=[xb, ss_b], w=[xs_b])
    for h in range(2):
        for j in range(4):
            kc = h * 4 + j
            tr(P, tps[h][:, j * 128:(j + 1) * 128], xs[:, kc * 128:(kc + 1) * 128], C.ident[:],
               r=[xs_b, C.ident_b], w=[tps_b[h]])
        src = tps[h][:].rearrange("p (j t) -> p j t", j=4)
        tt(P, "dve", outT[:, h * 4:(h + 1) * 4, col0:col0 + 128], src, gbT[:, h * 4:(h + 1) * 4, :], ALU.mult,
           r=[tps_b[h], gb_b], w=[outT_b])
        if outF is not None:
            tt(P, "dve", outF[:, h * 4:(h + 1) * 4, :], src, gbT[:, h * 4:(h + 1) * 4, :], ALU.mult,
               r=[tps_b[h], gb_b], w=[outF_b])


def load_gain(P, C, gvec_dram, gbT, es_name, gb_b=None):
    nc = P.nc
    P.dma("sp", C.gcol[:], gvec_dram.rearrange("(c p) -> p c", p=128), owner=C.gcol_b, w=[C.gcol_b], slow=True)
    gb_b = gb_b if gb_b is not None else C.gb_b
    for kc in range(NKC):
        ts(P, "pool", gbT[:, kc, :], C.ones_f[:, 0:128], C.gcol[:, kc:kc + 1], ALU.mult,
           r=[C.gcol_b, C.ones_b], w=[gb_b])


def phase_A(P, C, li):
    nc = P.nc
    es = contextlib.ExitStack()
    sb = lambda name, shape, dt: es.enter_context(nc.sbuf_tensor(name + "_L%d" % li, shape, dt))
    ps = lambda name, shape, dt: es.enter_context(nc.psum_tensor(name + "_L%d" % li, shape, dt))
    bufs = []

    def B(name, dma=False):
        b = P.buf(name, dma)
        bufs.append(b)
        return b

    Win = sb("A_win", [128, NKC, INW], BF16)
    Win_b = [B("A_win%d" % kc, True) for kc in range(NKC)]
    Wsw = sb("A_wsw", [128, NKC, 2 * DILW], BF16)
    Wsw_b = B("A_wsw")
    Wp = sb("A_wp", [128, 4, 128], BF16)
    Wp_b = B("A_wp", True)
    psc = sb("A_psc", [128, 4], F32)
    psc_b = B("A_psc", True)
    xt = [sb("A_xt0", [128, 4, D], F32)] * 2
    xt_b = [B("A_xt0", True)] * 2
    C.cosT = sb("A_cosT", [128, S], F32)
    C.sinT = sb("A_sinT", [128, S], F32)
    C.rope_b = B("rope")
    hT = [sb("A_hT%d" % i, [128, NKC, TT], BF16) for i in range(2)]
    hT_b = [B("A_hT%d" % i, True) for i in range(2)]
    junk = sb("A_junk", [128, D], BF16)
    junk_b = B("A_junk")
    ssq = [sb("A_ssq%d" % i, [128, 4], F32) for i in range(4)]
    ssq_b = [B("A_ssq%d" % i) for i in range(4)]
    xs = [sb("A_xs%d" % i, [128, D], F32) for i in range(2)]
    xs_b = [B("A_xs%d" % i) for i in range(2)]
    tps = [ps("A_tp%d" % i, [128, 512], F32) for i in range(4)]
    tps_b = [B("A_tp%d" % i) for i in range(4)]
    pj = [ps("A_pj%d" % i, [128, 512], F32) for i in range(4)]
    pj_b = [B("A_pj%d" % i) for i in range(4)]
    t1 = [sb("A_t1_%d" % i, [128, TT], F32) for i in range(2)]
    t1_b = [B("A_t1_%d" % i) for i in range(2)]
    t2 = [sb("A_t2_%d" % i, [128, TT], F32) for i in range(2)]
    t2_b = [B("A_t2_%d" % i) for i in range(2)]
    NST = 4
    stg = [sb("A_stg%d" % i, [128, TT], BF16) for i in range(NST)]
    stg_b = [B("A_stg%d" % i, True) for i in range(NST)]
    vst = [sb("A_vst%d" % i, [128, DILW + SBW], BF16) for i in range(2)]
    vst_b = [B("A_vst%d" % i, True) for i in range(2)]
    ub = [sb("A_ub%d" % g, [128, 16 + TT], F32) for g in range(4)]
    ub_b = [B("A_ub%d" % g) for g in range(4)]
    pw = [sb("A_pw%d" % i, [128, 16 + TT], F32) for i in range(2)]
    pw_b = [B("A_pw%d" % i) for i in range(2)]
    pl = [sb("A_pl%d" % i, [128, TT], BF16) for i in range(2)]
    pl_b = [B("A_pl%d" % i) for i in range(2)]

    build_rope(P, C, xt[0][:].rearrange("p b d -> p (b d)"), xt_b[0])
    w_in = C.w_in[li]
    for kc in range(NKC):
        P.dma("pool", Win[:, kc, :], w_in[kc * 128:(kc + 1) * 128, :], owner=Win_b[kc], w=[Win_b[kc]])
    P.dma("pool", Wp[:], C.w_pool[li].rearrange("g c d -> c g d"), owner=Wp_b, w=[Wp_b])
    P.dma("sp", psc[:], C.pool_scale[li].rearrange("(g p) -> p g", p=128), owner=psc_b, w=[psc_b], slow=True)
    load_gain(P, C, C.norm_mix[li], C.gbT, "A")
    for kc in range(NKC):
        src = Win[:, kc, 0:2 * DILW].rearrange("p (h two c) -> p h two c", two=2, c=32)
        dst = Wsw[:, kc, :].rearrange("p (h two c) -> p h two c", two=2, c=32)
        e = "dve" if kc % 2 == 0 else "pool"
        cp(P, e, dst[:, :, 0, :], src[:, :, 1, :], r=[Win_b[kc]], w=[Wsw_b])
        cp(P, e, dst[:, :, 1, :], src[:, :, 0, :], r=[Win_b[kc]], w=[Wsw_b])
    for g in range(4):
        P.op("pool", lambda g=g: nc.gpsimd.memset(ub[g][:, 0:16], 0.0), w=[ub_b[g]])

    xd = C.x_in if li == 0 else C.x_dram
    sti = 0
    pji = 0

    def load_x(t_):
        p_ = t_ % 2
        P.dma("sp", xt[p_][:], xd[t_ * TT:(t_ + 1) * TT, :].rearrange("(b p) d -> p b d", p=128),
              owner=xt_b[p_], r=[C.xd_b[t_]], w=[xt_b[p_]])

    def nA0(t_):
        p_ = t_ % 2
        for bi in range(4):
            q, q_b = ssq[bi], ssq_b[bi]
            act(P, junk[:], xt[p_][:, bi, :], AF.Square, r=[xt_b[p_]], w=[junk_b, q_b], accum_out=q[:, 0:1])
            act(P, q[:, 1:2], q[:, 0:1], AF.Ln, r=[q_b, C.cst_b], w=[q_b], scale=1.0 / D, bias=C.eps_col[:, 0:1])
            act(P, q[:, 2:3], q[:, 1:2], AF.Exp, r=[q_b], w=[q_b], scale=-0.5)

    def nA1(t_, bi):
        p_ = t_ % 2
        ts(P, "dve", xs[bi % 2][:], xt[p_][:, bi, :], ssq[bi][:, 2:3], ALU.mult, r=[xt_b[p_], ssq_b[bi]],
           w=[xs_b[bi % 2]])

    def nA2(t_, bi):
        for h_ in range(2):
            tp, tp_b = tps[(bi % 2) * 2 + h_], tps_b[(bi % 2) * 2 + h_]
            for j in range(4):
                kc = h_ * 4 + j
                tr(P, tp[:, j * 128:(j + 1) * 128], xs[bi % 2][:, kc * 128:(kc + 1) * 128], C.ident[:],
                   r=[xs_b[bi % 2], C.ident_b], w=[tp_b])

    def nA3(t_, bi):
        p_ = t_ % 2
        for h_ in range(2):
            tp, tp_b = tps[(bi % 2) * 2 + h_], tps_b[(bi % 2) * 2 + h_]
            tt(P, "dve", hT[p_][:, h_ * 4:(h_ + 1) * 4, bi * 128:(bi + 1) * 128],
               tp[:].rearrange("p (j t) -> p j t", j=4), C.gbT[:, h_ * 4:(h_ + 1) * 4, :], ALU.mult,
               r=[tp_b, C.gb_b], w=[hT_b[p_]])

    def norm_body(t_):
        nA1(t_, 0); nA1(t_, 1); nA2(t_, 0); nA3(t_, 0); nA1(t_, 2); nA2(t_, 1); nA3(t_, 1); nA1(t_, 3)
        nA2(t_, 2); nA3(t_, 2); nA2(t_, 3); nA3(t_, 3)
        if t_ + 1 < NTT:
            load_x(t_ + 1)

    load_x(0)
    nA0(0)
    norm_body(0)
    for ti in range(NTT):
        pb = ti % 2
        tok0 = ti * TT
        h = hT[pb]
        hb = hT_b[pb]
        P.dma("sp", C.HT[:, tok0:tok0 + TT].rearrange("(kc p) t -> p kc t", p=128), h[:], owner=hb, r=[hb],
              w=[C.HT_b])

        def proj_fm(col0, W=Win, Wb=None):
            nonlocal pji
            o = pj[pji % 4]
            ob = pj_b[pji % 4]
            pji += 1
            for kc in range(NKC):
                mm(P, o[:], W[:, kc, col0:col0 + 128], h[:, kc, :], kc == 0, kc == NKC - 1,
                   r=[hb, (Wb if Wb is not None else Win_b[kc])], w=[ob])
            return o, ob

        for which, cbase, dst in ((0, Q_D, C.QTd), (1, K_D, C.KTd)):
            if ti + 1 < NTT:
                if which == 1:
                    nA0(ti + 1)
            for ci in range(6):
                if which == 1 and ci == 4 and ti + 1 < NTT:
                    norm_body(ti + 1)
                a, a_b = proj_fm(cbase + ci * 128)
                s_, s_b = proj_fm(cbase + ci * 128, W=Wsw, Wb=Wsw_b)
                i2 = ci % 2
                tt(P, "dve", t1[i2][:], a[:], C.cosT[:, tok0:tok0 + TT], ALU.mult, r=[a_b, C.rope_b], w=[t1_b[i2]])
                tt(P, "dve", t2[i2][:], s_[:], C.sinT[:, tok0:tok0 + TT], ALU.mult, r=[s_b, C.rope_b], w=[t2_b[i2]])
                st = stg[sti % NST]
                st_b = stg_b[sti % NST]
                sti += 1
                tt(P, "pool", st[:], t1[i2][:], t2[i2][:], ALU.add, r=[t1_b[i2], t2_b[i2]], w=[st_b])
                P.dma("sp", dst[ci * 128:(ci + 1) * 128, tok0:tok0 + TT], st[:], owner=st_b, r=[st_b],
                      w=[C.QTd_b if which == 0 else C.KTd_b])
        for which, cbase, dst in ((0, Q_S, C.QTs), (1, K_S, C.KTs)):
            for ci in range(4):
                a, a_b = proj_fm(cbase + ci * 128)
                st = stg[sti % NST]
                st_b = stg_b[sti % NST]
                sti += 1
                cp(P, "act", st[:], a[:], r=[a_b], w=[st_b])
                P.dma("sp", dst[ci * 128:(ci + 1) * 128, tok0:tok0 + TT], st[:], owner=st_b, r=[st_b],
                      w=[C.QTs_b if which == 0 else C.KTs_b])
        pend = []
        for g in range(4):
            a, a_b = proj_fm(U_C + g * 128)
            u = ub[g]
            cp(P, "act", u[:, 16:16 + TT], a[:], r=[a_b], w=[ub_b[g]])
            wdw = POOLW[g]
            cur, cur_b = u, ub_b[g]
            sh = 1
            lvl = 0
            while sh < wdw:
                o = pw[lvl % 2]
                o_b = pw_b[lvl % 2]
                lo = 2 * sh - 1
                tt(P, "pool", o[:, lo:16 + TT], cur[:, lo:16 + TT], cur[:, lo - sh:16 + TT - sh], ALU.add,
                   r=[cur_b], w=[o_b])
                cur, cur_b = o, o_b
                sh *= 2
                lvl += 1
            o = pw[lvl % 2]
            o_b = pw_b[lvl % 2]
            if ti == 0:
                tt(P, "pool", o[:, 16:32], cur[:, 16:32], C.rcnt16[:, g, :], ALU.mult, r=[cur_b, C.rcnt_b], w=[o_b])
                ts(P, "pool", o[:, 32:16 + TT], cur[:, 32:16 + TT], 1.0 / wdw, ALU.mult, r=[cur_b], w=[o_b])
            else:
                ts(P, "pool", o[:, 16:16 + TT], cur[:, 16:16 + TT], 1.0 / wdw, ALU.mult, r=[cur_b], w=[o_b])
            i2 = g % 2
            tt(P, "pool", pl[i2][:], o[:, 16:16 + TT], u[:, 16:16 + TT], ALU.subtract, r=[o_b, ub_b[g]], w=[pl_b[i2]])
            cp(P, "pool", u[:, 0:16], u[:, TT:TT + 16], r=[ub_b[g]], w=[ub_b[g]])

            def p2(g=g, i2=i2, tok0=tok0):
                nonlocal pji, sti
                y = pj[pji % 4]
                y_b = pj_b[pji % 4]
                pji += 1
                mm(P, y[:], Wp[:, g, :], pl[i2][:], True, True, r=[Wp_b, pl_b[i2]], w=[y_b])
                st = stg[sti % NST]
                st_b = stg_b[sti % NST]
                sti += 1
                ts(P, "dve", st[:], y[:], psc[:, g:g + 1], ALU.mult, r=[y_b, psc_b], w=[st_b])
                P.dma("sp", C.YCT[g * 128:(g + 1) * 128, tok0:tok0 + TT], st[:], owner=st_b, r=[st_b],
                      w=[C.YCT_b])

            if pend:
                pend.pop()()
            pend.append(p2)
        for bi in range(4):
            vs = vst[bi % 2]
            vs_b = vst_b[bi % 2]
            for (c0, cw, o0) in ((V_D, 512, 0), (V_D + 512, 256, 512), (V_S, 512, DILW)):
                o = pj[pji % 4]
                ob = pj_b[pji % 4]
                pji += 1
                for kc in range(NKC):
                    mm(P, o[:, 0:cw], h[:, kc, bi * 128:(bi + 1) * 128], Win[:, kc, c0:c0 + cw], kc == 0,
                       kc == NKC - 1, r=[hb, Win_b[kc]], w=[ob])
                cp(P, "act" if o0 != 512 else "dve", vs[:, o0:o0 + cw], o[:, 0:cw], r=[ob], w=[vs_b])
                if pend:
                    pend.pop()()
            t0 = tok0 + bi * 128
            P.dma("sp", C.Vd[t0:t0 + 128, :], vs[:, 0:DILW], owner=vs_b, r=[vs_b], w=[C.Vd_b])
            P.dma("sp", C.Vs[t0:t0 + 128, :], vs[:, DILW:DILW + SBW], owner=vs_b, r=[vs_b], w=[C.Vs_b])
    P.barrier()
    es.close()
    P.release(bufs)


def build_rope(P, C, pi_f32_tile, pi_b):
    nc = P.nc
    pi = pi_f32_tile.bitcast(I32)
    P.dma("sp", pi, C.positions.partition_broadcast(128), owner=pi_b, w=[pi_b])
    cp(P, "dve", C.cosT[:], pi, r=[pi_b], w=[C.rope_b])
    A_ = C.sinT
    T_ = C.cosT
    ts(P, "dve", A_[:], T_[:], C.invf[:, 0:1], ALU.mult, r=[C.rope_b, C.cst_b], w=[C.rope_b])
    ts(P, "dve", T_[:], A_[:], float(1.0 / (2 * np.pi)), ALU.mult, r=[C.rope_b], w=[C.rope_b])
    cp(P, "dve", pi, T_[:], r=[C.rope_b], w=[pi_b])
    cp(P, "dve", T_[:], pi, r=[pi_b], w=[C.rope_b])
    C1 = 6.28125
    C2 = float(2 * np.pi - 6.28125)
    stt(P, "dve", A_[:], T_[:], -C1, A_[:], ALU.mult, ALU.add, r=[C.rope_b], w=[C.rope_b])
    stt(P, "dve", A_[:], T_[:], -C2, A_[:], ALU.mult, ALU.add, r=[C.rope_b], w=[C.rope_b])
    ts(P, "dve", T_[:], A_[:], float(np.pi / 2), ALU.is_gt, r=[C.rope_b], w=[C.rope_b])
    stt(P, "dve", T_[:], T_[:], float(-2 * np.pi), A_[:], ALU.mult, ALU.add, r=[C.rope_b], w=[C.rope_b])
    LIM = 3.1415925
    ts(P, "dve", T_[:], T_[:], float(np.pi / 2), ALU.add, r=[C.rope_b], w=[C.rope_b], s2=LIM, op1=ALU.min)
    ts(P, "dve", T_[:], T_[:], -LIM, ALU.max, r=[C.rope_b], w=[C.rope_b])
    ts(P, "dve", A_[:], A_[:], LIM, ALU.min, r=[C.rope_b], w=[C.rope_b], s2=-LIM, op1=ALU.max)
    act(P, C.cosT[:], T_[:], AF.Sin, r=[C.rope_b], w=[C.rope_b])
    act(P, C.sinT[:], A_[:], AF.Sin, r=[C.rope_b], w=[C.rope_b])
    ts(P, "dve", C.sinT[:], C.sinT[:], C.sgn[:, 0:1], ALU.mult, r=[C.rope_b, C.cst_b], w=[C.rope_b])


def phase_B_sb(P, C, li):
    nc = P.nc
    es = contextlib.ExitStack()
    sb = lambda name, shape, dt: es.enter_context(nc.sbuf_tensor(name + "_L%d" % li, shape, dt))
    ps = lambda name, shape, dt: es.enter_context(nc.psum_tensor(name + "_L%d" % li, shape, dt))
    bufs = []

    def B(name, dma=False):
        b = P.buf(name, dma)
        bufs.append(b)
        return b

    KT = [sb("S_KT%d" % i, [64, S], BF16) for i in range(2)]
    QT = [sb("S_QT%d" % i, [64, S], BF16) for i in range(2)]
    V = [sb("S_V%d" % i, [128, 32, 64], BF16) for i in range(2)]
    hd_b = [B("S_hd%d" % i, True) for i in range(2)]
    sp = [sb("S_sp%d" % i, [128, S], F32) for i in range(2)]
    sp_b = [[B("S_sp%d_%d" % (i, c)) for c in range(8)] for i in range(2)]
    ND = 3
    dd = [sb("S_d%d" % i, [128, S], F32) for i in range(ND)]
    dd_b = [B("S_d%d" % i) for i in range(ND)]
    G = [sb("S_G%d" % i, [128, S], F32) for i in range(2)]
    G_b = [B("S_G%d" % i) for i in range(2)]
    NN = 4
    nt = [sb("S_nt%d" % i, [128, 2], F32) for i in range(NN)]
    nt_b = [B("S_nt%d" % i) for i in range(NN)]
    A = [sb("S_A%d" % i, [128, S], BF16) for i in range(2)]
    A_b = [B("S_A%d" % i) for i in range(2)]
    AT = [sb("S_AT%d" % i, [128, S], BF16) for i in range(2)]
    AT_b = [B("S_AT%d" % i) for i in range(2)]
    NOT = 1
    OT = [sb("S_OT%d" % i, [64, S], BF16) for i in range(NOT)]
    OT_b = [B("S_OT%d" % i, True) for i in range(NOT)]
    NZ = 4
    NE = 3
    et = [sb("S_et%d" % i, [128, 512], F32) for i in range(NE)]
    et_b = [B("S_et%d" % i) for i in range(NE)]
    trif = sb("S_trif", [128, 128], F32)
    trib = sb("S_trib", [128, 128], BF16)
    idb = sb("S_idb", [128, 128], BF16)
    tri_b = B("S_tri", True)
    zps = [ps("S_z%d" % i, [128, 512], F32) for i in range(NZ)]
    zps_b = [B("S_z%d" % i) for i in range(NZ)]
    tpp = [ps("S_tp%d" % i, [128, 512], BF16) for i in range(2)]
    tpp_b = [B("S_tp%d" % i) for i in range(2)]
    ots = [ps("S_o%d" % i, [64, 128], F32) for i in range(2)]
    ots_b = [B("S_o%d" % i) for i in range(2)]

    P.dma("sp", trif[:], C.tri_in[:, :], owner=tri_b, w=[tri_b])
    mneg = trib
    ts(P, "dve", mneg[:], trif[:], 30000.0, ALU.mult, r=[tri_b], w=[tri_b], s2=-30000.0, op1=ALU.add)
    cp(P, "dve", idb[:], C.ident[:], r=[C.ident_b], w=[tri_b])

    def load_head(hd):
        i = hd % 2
        P.dma("sp", KT[i][:], C.KTs[hd * 64:(hd + 1) * 64, :], owner=hd_b[i], r=[C.KTs_b], w=[hd_b[i]])
        P.dma("sp", QT[i][:], C.QTs[hd * 64:(hd + 1) * 64, :], owner=hd_b[i], r=[C.QTs_b], w=[hd_b[i]])
        P.dma("sp", V[i][:], C.Vs[:, hd * 64:(hd + 1) * 64].rearrange("(b p) c -> p b c", p=128), owner=hd_b[i],
              r=[C.Vs_b], w=[hd_b[i]])

    blocks = [(hd, qb) for hd in range(8) for qb in range(32)]
    NBLK = len(blocks)
    cnt = {"z": 0, "t": 0, "o": 0}

    def s1(k):
        hd, qb = blocks[k]
        i = hd % 2
        rb = k % 2
        db = k % ND
        nk = 128 * (qb + 1)
        nkc = (nk + 511) // 512
        pend = None
        for kc in range(nkc):
            w = min(512, nk - kc * 512)
            zi = cnt["z"]
            cnt["z"] += 1
            z = zps[zi % NZ]
            z_b = zps_b[zi % NZ]
            e = et[zi % NE]
            e_b = et_b[zi % NE]
            q_ = QT[i][:, qb * 128:(qb + 1) * 128]
            if kc < nkc - 1:
                mm(P, z[:, :w], q_, KT[i][:, kc * 512:kc * 512 + w], True, True, r=[hd_b[i]], w=[z_b])
            else:
                if w > 128:
                    mm(P, z[:, :w - 128], q_, KT[i][:, kc * 512:kc * 512 + w - 128], True, True, r=[hd_b[i]], w=[z_b])
                mm(P, z[:, w - 128:w], q_, KT[i][:, kc * 512 + w - 128:kc * 512 + w], True, False, r=[hd_b[i]],
                   w=[z_b])
                mm(P, z[:, w - 128:w], idb[:], mneg[:], False, True, r=[tri_b], w=[z_b])
            act(P, e[:, :w], z[:, :w], AF.Exp, r=[z_b], w=[e_b], scale=0.125)

            def tail(kc=kc, w=w, z=z, z_b=z_b, e=e, e_b=e_b):
                act(P, sp[rb][:, kc * 512:kc * 512 + w], e[:, :w], AF.Ln, r=[e_b, C.cst_b], w=[sp_b[rb][kc]],
                    bias=C.one_col[:, 0:1])
                stt(P, "dve", dd[db][:, kc * 512:kc * 512 + w], z[:, :w], 0.125, sp[rb][:, kc * 512:kc * 512 + w],
                    ALU.mult, ALU.subtract, r=[z_b, sp_b[rb][kc]], w=[dd_b[db]])

            if pend is not None:
                pend()
            pend = tail
        if pend is not None:
            pend()

    def s2a(k):
        hd, qb = blocks[k]
        rb = k % 2
        nk = 128 * (qb + 1)
        nkc = (nk + 511) // 512
        g_ = G[k % 2]
        g_b = G_b[k % 2]
        n_ = nt[k % NN]
        n_b = nt_b[k % NN]
        spb = sp_b[rb][:nkc]
        P.op("dve", lambda: nc.vector.tensor_tensor_scan(
            out=g_[:, :nk], data0=sp[rb][:, :nk], data1=sp[rb][:, :nk], initial=0.0, op0=ALU.add, op1=ALU.max),
            r=spb, w=[g_b])
        ts(P, "dve", n_[:, 0:1], g_[:, nk - 1:nk], -1.0, ALU.mult, r=[g_b], w=[n_b])

    def s2b(k):
        hd, qb = blocks[k]
        db = k % ND
        nk = 128 * (qb + 1)
        tt(P, "pool", dd[db][:, :nk], dd[db][:, :nk], G[k % 2][:, :nk], ALU.add, r=[dd_b[db], G_b[k % 2]],
           w=[dd_b[db]])

    def s2c(k):
        hd, qb = blocks[k]
        rb = k % 2
        db = k % ND
        nk = 128 * (qb + 1)
        n_ = nt[k % NN]
        n_b = nt_b[k % NN]
        act(P, A[rb][:, :nk], dd[db][:, :nk], AF.Exp, r=[dd_b[db], n_b], w=[A_b[rb]], bias=n_[:, 0:1])

    def s3(k):
        hd, qb = blocks[k]
        i = hd % 2
        rb = k % 2
        ot = OT[hd % NOT]
        ot_b = OT_b[hd % NOT]
        for kb4 in range(0, qb + 1, 4):
            n4 = min(4, qb + 1 - kb4)
            ti = cnt["t"]
            cnt["t"] += 1
            tp = tpp[ti % 2]
            tp_b = tpp_b[ti % 2]
            for j in range(n4):
                tr(P, tp[:, j * 128:(j + 1) * 128], A[rb][:, (kb4 + j) * 128:(kb4 + j + 1) * 128], idb[:],
                   r=[A_b[rb], tri_b], w=[tp_b])
            cp(P, "dve" if ti % 2 == 0 else "act", AT[rb][:, kb4 * 128:(kb4 + n4) * 128], tp[:, :n4 * 128],
               r=[tp_b], w=[AT_b[rb]])

    def s3b(k):
        hd, qb = blocks[k]
        i = hd % 2
        rb = k % 2
        o = ots[k % 2]
        o_b = ots_b[k % 2]
        for kb in range(qb + 1):
            mm(P, o[:, :], V[i][:, kb, :], AT[rb][:, kb * 128:(kb + 1) * 128], kb == 0, kb == qb,
               r=[hd_b[i], AT_b[rb]], w=[o_b])

    def s4(k):
        hd, qb = blocks[k]
        ot = OT[hd % NOT]
        ot_b = OT_b[hd % NOT]
        o = ots[k % 2]
        o_b = ots_b[k % 2]
        cp(P, "act", ot[:, qb * 128:(qb + 1) * 128], o[:, :], r=[o_b], w=[ot_b])
        if qb == 31:
            P.dma("sp", C.OBT[hd * 64:(hd + 1) * 64, :], ot[:], owner=ot_b, r=[ot_b], w=[C.OBT_b])

    load_head(0)
    for step in range(NBLK + 5):
        for lag, fn in ((4, s3), (3, s2c), (2, s2b), (1, s2a), (0, s1), (4, s3b), (5, s4)):
            k = step - lag
            if 0 <= k < NBLK:
                fn(k)
        if step % 32 == 5 and step // 32 + 1 < 8:
            load_head(step // 32 + 1)
    P.barrier()
    es.close()
    P.release(bufs)


def phase_B_dil(P, C, li):
    nc = P.nc
    es = contextlib.ExitStack()
    sb = lambda name, shape, dt: es.enter_context(nc.sbuf_tensor(name + "_L%d" % li, shape, dt))
    ps = lambda name, shape, dt: es.enter_context(nc.psum_tensor(name + "_L%d" % li, shape, dt))
    bufs = []

    def B(name, dma=False):
        b = P.buf(name, dma)
        bufs.append(b)
        return b

    KT = [sb("L_KT%d" % i, [64, S], BF16) for i in range(2)]
    QT = [sb("L_QT%d" % i, [64, S], BF16) for i in range(2)]
    Vr = [sb("L_V%d" % i, [128, 32, 64], BF16) for i in range(2)]
    hd_b = [B("L_hd%d" % i, True) for i in range(2)]
    NDs = [sb("L_ND%d" % i, [64, 2, S], F32) for i in range(2)]
    NDs_b = [B("L_ND%d" % i) for i in range(2)]
    OA = sb("L_OA", [64, S], BF16)
    OA_b = B("L_OA", True)
    mbf = sb("L_mbf", [128, 256], F32)
    mbb = sb("L_mbb", [128, 256], BF16)
    onb = sb("L_onb", [128, 64], BF16)
    idb = sb("L_idb", [128, 128], BF16)
    mb_b = B("L_mb", True)
    Pm = [sb("L_P%d" % i, [128, 512], BF16) for i in range(3)]
    Pm_b = [B("L_P%d" % i) for i in range(3)]
    stp = [ps("L_st%d" % i, [128, 512], F32) for i in range(3)]
    stp_b = [B("L_st%d" % i) for i in range(3)]
    ndp = [ps("L_nd%d" % i, [64, 512], F32) for i in range(3)]
    ndp_b = [B("L_nd%d" % i) for i in range(3)]

    P.dma("sp", mbf[:], C.mband_in[:, :], owner=mb_b, w=[mb_b])
    ts(P, "dve", mbb[:], mbf[:], 30000.0, ALU.mult, r=[mb_b], w=[mb_b], s2=-30000.0, op1=ALU.add)
    cp(P, "dve", idb[:], C.ident[:], r=[C.ident_b], w=[mb_b])
    P.op("dve", lambda: nc.vector.memset(onb[:], 1.0), w=[mb_b])

    heads = [(j, g) for j in range(4) for g in range(3)]

    def load_head(idx):
        j, g = heads[idx]
        d = DIL[g][1]
        hh = g * 4 + j
        i = idx % 2
        P.dma("sp", KT[i][:], C.KTd[hh * 64:(hh + 1) * 64, :], owner=hd_b[i], r=[C.KTd_b], w=[hd_b[i]])
        P.dma("sp", QT[i][:], C.QTd[hh * 64:(hh + 1) * 64, :], owner=hd_b[i], r=[C.QTd_b], w=[hd_b[i]])
        nbn = 32 // d
        src = C.Vd[:, hh * 64:(hh + 1) * 64].rearrange("(nb i r) c -> r i nb c", i=128, r=d)
        for r in range(d):
            P.dma("sp", Vr[i][:, r * nbn:(r + 1) * nbn, :], src[r], owner=hd_b[i], r=[C.Vd_b], w=[hd_b[i]])

    blocks = []
    first_of = {}
    last_of = {}
    for idx, (j, g) in enumerate(heads):
        d = DIL[g][1]
        nbn = 32 // d
        first_of[idx] = len(blocks)
        for r in range(d):
            for p_ in range(nbn // 2):
                blocks.append((idx, r, p_))
        last_of[idx] = len(blocks) - 1
    NBLK = len(blocks)

    def geom(k):
        idx, r, p_ = blocks[k]
        j, g = heads[idx]
        d = DIL[g][1]
        nbn = 32 // d
        return idx, r, p_, j, g, d, nbn

    def qsl(base, d, n=128):
        return slice(base, base + (n - 1) * d + 1, d) if d > 1 else slice(base, base + n)

    def dA(k):
        idx, r, p_, j, g, d, nbn = geom(k)
        i = idx % 2
        st = stp[k % 3]
        st_b = stp_b[k % 3]
        for b in range(2):
            nb = 2 * p_ + b
            base = nb * 128 * d + r
            sl = qsl(base, d)
            o = b * 256
            mm(P, st[:, o:o + 128], KT[i][:, sl], QT[i][:, sl], True, False, r=[hd_b[i]], w=[st_b])
            mm(P, st[:, o:o + 128], idb[:], mbb[:, 0:128], False, True, r=[mb_b], w=[st_b])
            if nb > 0:
                slp = qsl(base - 128 * d, d)
                mm(P, st[:, o + 128:o + 256], KT[i][:, slp], QT[i][:, sl], True, False, r=[hd_b[i]], w=[st_b])
                mm(P, st[:, o + 128:o + 256], idb[:], mbb[:, 128:256], False, True, r=[mb_b], w=[st_b])

    def dB(k):
        idx, r, p_, j, g, d, nbn = geom(k)
        st, st_b, pm, pm_b = stp[k % 3], stp_b[k % 3], Pm[k % 3], Pm_b[k % 3]
        if p_ == 0:
            act(P, pm[:, 0:128], st[:, 0:128], AF.Exp, r=[st_b], w=[pm_b], scale=0.125)
            act(P, pm[:, 256:512], st[:, 256:512], AF.Exp, r=[st_b], w=[pm_b], scale=0.125)
        else:
            act(P, pm[:, 0:512], st[:, 0:512], AF.Exp, r=[st_b], w=[pm_b], scale=0.125)

    def dC(k):
        idx, r, p_, j, g, d, nbn = geom(k)
        i = idx % 2
        pm, pm_b, nd, nd_b = Pm[k % 3], Pm_b[k % 3], ndp[k % 3], ndp_b[k % 3]
        for b in range(2):
            nb = 2 * p_ + b
            nt_ = 2 if nb > 0 else 1
            o = b * 256
            for t in range(nt_):
                tidx = r * nbn + nb - t
                mm(P, nd[:, b * 128:(b + 1) * 128], Vr[i][:, tidx, :], pm[:, o + t * 128:o + (t + 1) * 128], t == 0,
                   t == nt_ - 1, r=[hd_b[i], pm_b], w=[nd_b])
            for t in range(nt_):
                mm(P, nd[:, 256 + b * 128:256 + (b + 1) * 128], onb[:, :], pm[:, o + t * 128:o + (t + 1) * 128],
                   t == 0, t == nt_ - 1, r=[mb_b, pm_b], w=[nd_b])

    def dD(k):
        idx, r, p_, j, g, d, nbn = geom(k)
        nd, nd_b = ndp[k % 3], ndp_b[k % 3]
        ND, ND_b = NDs[j % 2], NDs_b[j % 2]
        base = 2 * p_ * 128 * d + r
        dst = ND[:, :, qsl(base, d, 256)]
        src = nd[:, :].rearrange("p (a q) -> p a q", a=2)
        if g == 0:
            cp(P, "dve", dst, src, r=[nd_b], w=[ND_b])
        else:
            tt(P, "dve", dst, src, dst, ALU.add, r=[nd_b, ND_b], w=[ND_b])
        if k == last_of[idx] and g == 2:
            act(P, ND[:, 1, :], ND[:, 1, :], AF.Ln, r=[ND_b], w=[ND_b])
            act(P, ND[:, 1, :], ND[:, 1, :], AF.Exp, r=[ND_b], w=[ND_b], scale=-1.0)
            tt(P, "pool", OA[:], ND[:, 0, :], ND[:, 1, :], ALU.mult, r=[ND_b], w=[OA_b])
            P.dma("sp", C.OAT[j * 64:(j + 1) * 64, :], OA[:], owner=OA_b, r=[OA_b], w=[C.OAT_b])

    load_head(0)
    for step in range(NBLK + 3):
        for lag, fn in ((3, dD), (2, dC), (1, dB), (0, dA)):
            k = step - lag
            if 0 <= k < NBLK:
                fn(k)
        if step < NBLK:
            idx = blocks[step][0]
            if step == min(first_of[idx] + 3, last_of[idx]) and idx + 1 < len(heads):
                load_head(idx + 1)
    P.barrier()
    es.close()
    P.release(bufs)


class Ring:
    def __init__(self, P, tiles, name):
        self.t = tiles
        self.b = [P.buf("%s%d" % (name, i)) for i in range(len(tiles))]
        self.i = 0

    def next(self):
        k = self.i % len(self.t)
        self.i += 1
        return self.t[k], self.b[k]


def phase_C(P, C, li):
    nc = P.nc
    es = contextlib.ExitStack()
    sb = lambda name, shape, dt: es.enter_context(nc.sbuf_tensor(name + "_L%d" % li, shape, dt))
    ps = lambda name, shape, dt: es.enter_context(nc.psum_tensor(name + "_L%d" % li, shape, dt))
    bufs = []

    def B(name, dma=False):
        b = P.buf(name, dma)
        bufs.append(b)
        return b

    Wua = sb("C_wua", [128, 2, D], BF16); Wua_b = B("C_wua", True)
    Wub = sb("C_wub", [128, 4, D], BF16); Wub_b = B("C_wub", True)
    Wuc = sb("C_wuc", [128, 4, D], BF16); Wuc_b = B("C_wuc", True)
    Wg = sb("C_wg", [128, NKC, 3 * D], BF16); Wg_b = B("C_wg", True)
    Wo = sb("C_wo", [128, NKC, D], BF16); Wo_b = B("C_wo", True)
    bg = sb("C_bg", [128, 24], F32); bg_b = B("C_bg", True)
    hT = [sb("C_hT%d" % i, [128, NKC, TT], BF16) for i in range(2)]
    oa = [sb("C_oa%d" % i, [128, 2, TT], BF16) for i in range(2)]
    ob = [sb("C_ob%d" % i, [128, 4, TT], BF16) for i in range(2)]
    yc = [sb("C_yc%d" % i, [128, 4, TT], BF16) for i in range(2)]
    in_b = [B("C_in%d" % i, True) for i in range(2)]
    xt = [sb("C_xt%d" % i, [128, 4, D], F32) for i in range(2)]
    xt_b = [B("C_xt%d" % i, True) for i in range(2)]
    mT = [sb("C_mT%d" % i, [128, NKC, TT], BF16) for i in range(2)]
    mT_b = [B("C_mT%d" % i) for i in range(2)]
    gs = [sb("C_gs%d" % i, [128, TT], F32) for i in range(3)]
    gs_b = [B("C_gs%d" % i) for i in range(3)]
    macc = [sb("C_ma%d" % i, [128, TT], F32) for i in range(2)]
    macc_b = [B("C_ma%d" % i) for i in range(2)]
    tmp = [sb("C_tm%d" % i, [128, TT], F32) for i in range(2)]
    tmp_b = [B("C_tm%d" % i) for i in range(2)]
    ring = Ring(P, [ps("C_ps%d" % i, [128, 512], F32) for i in range(8)], "C_ps")

    P.dma("pool", Wua[:], C.w_up_a[li].rearrange("(kc p) d -> p kc d", p=128), owner=Wua_b, w=[Wua_b])
    P.dma("pool", Wub[:], C.w_up_b[li].rearrange("(kc p) d -> p kc d", p=128), owner=Wub_b, w=[Wub_b])
    P.dma("pool", Wuc[:], C.w_up_c[li].rearrange("(kc p) d -> p kc d", p=128), owner=Wuc_b, w=[Wuc_b])
    for kc in range(NKC):
        P.dma("pool", Wg[:, kc, :], C.w_gate[li][kc * 128:(kc + 1) * 128, :], owner=Wg_b, w=[Wg_b])
    for kc in range(NKC):
        P.dma("pool", Wo[:, kc, :], C.w_out[li][kc * 128:(kc + 1) * 128, :], owner=Wo_b, w=[Wo_b])
    P.dma("sp", bg[:], C.b_gate[li].rearrange("(c p) -> p c", p=128), owner=bg_b, w=[bg_b], slow=True)

    xd = C.x_in if li == 0 else C.x_dram
    fm = lambda ap: ap.rearrange("(kc p) t -> p kc t", p=128)

    def load(ti):
        pb = ti % 2
        t0 = ti * TT
        P.dma("sp", hT[pb][:], fm(C.HT[:, t0:t0 + TT]), owner=in_b[pb], r=[C.HT_b], w=[in_b[pb]])
        P.dma("sp", oa[pb][:], fm(C.OAT[:, t0:t0 + TT]), owner=in_b[pb], r=[C.OAT_b], w=[in_b[pb]])
        P.dma("sp", ob[pb][:], fm(C.OBT[:, t0:t0 + TT]), owner=in_b[pb], r=[C.OBT_b], w=[in_b[pb]])
        P.dma("sp", yc[pb][:], fm(C.YCT[:, t0:t0 + TT]), owner=in_b[pb], r=[C.YCT_b], w=[in_b[pb]])
        P.dma("sp", xt[pb][:], xd[t0:t0 + TT, :].rearrange("(b p) d -> p b d", p=128), owner=xt_b[pb],
              r=[C.xd_b[ti]], w=[xt_b[pb]])

    load(0)
    gi = 0
    ai = 0
    for ti in range(NTT):
        pb = ti % 2
        t0 = ti * TT
        if ti + 1 < NTT:
            load(ti + 1)
        for fc in range(NKC):
            ma = macc[ai % 2]
            ma_b = macc_b[ai % 2]
            ai += 1
            for j, (Wu, Wu_b, nk, src) in enumerate(((Wua, Wua_b, 2, oa), (Wub, Wub_b, 4, ob), (Wuc, Wuc_b, 4, yc))):
                py, py_b = ring.next()
                for kc in range(nk):
                    mm(P, py[:], Wu[:, kc, fc * 128:(fc + 1) * 128], src[pb][:, kc, :], kc == 0, kc == nk - 1,
                       r=[Wu_b, in_b[pb]], w=[py_b])
                pg, pg_b = ring.next()
                c0 = j * D + fc * 128
                for kc in range(NKC):
                    mm(P, pg[:], Wg[:, kc, c0:c0 + 128], hT[pb][:, kc, :], kc == 0, kc == NKC - 1,
                       r=[Wg_b, in_b[pb]], w=[pg_b])
                g = gs[gi % 3]
                g_b = gs_b[gi % 3]
                gi += 1
                act(P, g[:], pg[:], AF.Sigmoid, r=[pg_b, bg_b], w=[g_b], bias=bg[:, j * 8 + fc:j * 8 + fc + 1])
                if j == 0:
                    tt(P, "dve", ma[:], py[:], g[:], ALU.mult, r=[py_b, g_b], w=[ma_b])
                else:
                    tm = tmp[j % 2]
                    tm_b = tmp_b[j % 2]
                    tt(P, "dve", tm[:], py[:], g[:], ALU.mult, r=[py_b, g_b], w=[tm_b])
                    if j == 1:
                        tt(P, "pool", ma[:], ma[:], tm[:], ALU.add, r=[ma_b, tm_b], w=[ma_b])
                    else:
                        tt(P, "pool", mT[pb][:, fc, :], ma[:], tm[:], ALU.add, r=[ma_b, tm_b], w=[mT_b[pb]])
        for bi in range(4):
            for hf in range(2):
                po, po_b = ring.next()
                for kc in range(NKC):
                    mm(P, po[:], mT[pb][:, kc, bi * 128:(bi + 1) * 128], Wo[:, kc, hf * 512:(hf + 1) * 512], kc == 0,
                       kc == NKC - 1, r=[mT_b[pb], Wo_b], w=[po_b])
                xs_ = xt[pb][:, bi, hf * 512:(hf + 1) * 512]
                tt(P, "dve", xs_, po[:], xs_, ALU.add, r=[po_b, xt_b[pb]], w=[xt_b[pb]])
        P.dma("sp", C.x1_dram[t0:t0 + TT, :].rearrange("(b p) d -> p b d", p=128), xt[pb][:], owner=xt_b[pb],
              r=[xt_b[pb]], w=[C.x1d_b])
    P.barrier()
    es.close()
    P.release(bufs)


def phase_D(P, C, li):
    nc = P.nc
    es = contextlib.ExitStack()
    sb = lambda name, shape, dt: es.enter_context(nc.sbuf_tensor(name + "_L%d" % li, shape, dt))
    ps = lambda name, shape, dt: es.enter_context(nc.psum_tensor(name + "_L%d" % li, shape, dt))
    bufs = []

    def B(name, dma=False):
        b = P.buf(name, dma)
        bufs.append(b)
        return b

    last = (li == DEPTH - 1)
    NB = TS // 128
    NT5 = TS // 512
    yacc = sb("D_y", [128, NB, D], F32)
    y_b = [B("D_y%d" % i) for i in range(NB)]
    yld_b = [B("D_yld%d" % i, True) for i in range(4)]
    hmT = sb("D_hmT", [128, NKC, TS], BF16)
    hm_b = [B("D_hm%d" % i) for i in range(NB)]
    hfT = [sb("D_hfT%d" % i, [128, NKC, 128], F32) for i in range(2)]
    hf_b = [B("D_hf%d" % i) for i in range(2)]
    NW = 2
    Weg = [sb("D_weg%d" % i, [128, NKC, EH], BF16) for i in range(NW)]
    Weu = [sb("D_weu%d" % i, [128, NKC, EH], BF16) for i in range(NW)]
    Wed = [sb("D_wed%d" % i, [128, 2, D], BF16) for i in range(NW)]
    We_b = [B("D_we%d" % i, True) for i in range(NW)]
    Wr = sb("D_wr", [128, NKC, 20], F32); Wr_b = B("D_wr", True)
    br = sb("D_br", [128, 20], F32); br_b = B("D_br", True)
    Wpg = sb("D_wpg", [128, NKC, D], BF16); Wpg_b = B("D_wpg", True)
    Wpi = sb("D_wpi", [128, 2, D], BF16); Wpi_b = B("D_wpi", True)
    gbT2 = sb("D_gbT2", [128, NKC, 128], F32); gb2_b = B("D_gb2")
    gate = sb("D_gate", [128, NB, 16], F32)
    gate_b = [B("D_gate%d" % i) for i in range(NB)]
    rt = sb("D_rt", [128, 64], F32); rt_b = B("D_rt")
    junk = sb("D_junk", [128, D], BF16); junk_b = B("D_junk")
    ss = sb("D_ss", [128, 4], F32); ss_b = B("D_ss")
    ssr = [sb("D_ssr%d" % i, [128, 4], F32) for i in range(3)]
    ssr_b = [B("D_ssr%d" % i) for i in range(3)]
    xs = [sb("D_xs%d" % i, [128, D], F32) for i in range(2)]
    xs_b = [B("D_xs%d" % i) for i in range(2)]
    hid = [sb("D_hid%d" % i, [128, 2, 512], BF16) for i in range(2)]
    hid_b = [[B("D_hid%d_%d" % (i, f)) for f in range(2)] for i in range(2)]
    sl = [sb("D_sl%d" % i, [128, 512], F32) for i in range(4)]
    sl_b = [B("D_sl%d" % i) for i in range(4)]
    hpT = [sb("D_hpT%d" % i, [128, NKC, 128], BF16) for i in range(2)]
    hp_b = [B("D_hp%d" % i) for i in range(2)]
    pin = [sb("D_pin%d" % i, [128, PLE], F32) for i in range(2)]
    pin_b = [B("D_pin%d" % i, True) for i in range(2)]
    pT = [sb("D_pT%d" % i, [128, 2, 128], BF16) for i in range(2)]
    pT_b = [B("D_pT%d" % i) for i in range(2)]
    sg = [sb("D_sg%d" % i, [128, 512], F32) for i in range(2)]
    sg_b = [B("D_sg%d" % i) for i in range(2)]
    junk2 = sb("D_junk2", [128, D], BF16); junk2_b = B("D_junk2")
    ss2 = sb("D_ss2", [128, 4], F32); ss2_b = B("D_ss2")
    st_b = [B("D_st%d" % i, True) for i in range(4)]
    if last:
        gfin = sb("D_gfin", [128, D], F32); gfin_b = B("D_gfin", True)
        ob_ = [sb("D_ob0", [128, D], F32)] * 2
        ob_b = [B("D_ob0", True)] * 2
    ring = Ring(P, [ps("D_ps%d" % i, [128, 512], F32) for i in range(8)], "D_ps")

    P.dma("sp", Wr[:, :, 0:4], C.w_rg[li].rearrange("(kc p) g -> p kc g", p=128), owner=Wr_b, w=[Wr_b], slow=True)
    for g in range(4):
        P.dma("sp", Wr[:, :, 4 + 4 * g:8 + 4 * g], C.w_re[li][g].rearrange("(kc p) e -> p kc e", p=128), owner=Wr_b,
              w=[Wr_b], slow=True)
    P.dma("sp", br[:, 0:4], C.b_rg[li].partition_broadcast(128), owner=br_b, w=[br_b])
    P.dma("sp", br[:, 4:20], C.b_re[li].partition_broadcast(128), owner=br_b, w=[br_b])
    for kc in range(NKC):
        P.dma("pool", Wpg[:, kc, :], C.w_pg[li][kc * 128:(kc + 1) * 128, :], owner=Wpg_b, w=[Wpg_b])
    P.dma("pool", Wpi[:], C.w_pi[li].rearrange("(kc p) d -> p kc d", p=128), owner=Wpi_b, w=[Wpi_b])
    load_gain(P, C, C.norm_moe[li], C.gbT, "D")
    load_gain(P, C, C.norm_ple[li], gbT2, "D2", gb_b=gb2_b)
    if last:
        P.dma("sp", gfin[:], C.norm_final.partition_broadcast(128), owner=gfin_b, w=[gfin_b])

    def load_expert(k):
        e = k % NEXP
        i = k % NW
        P.dma("pool", Weg[i][:], C.w_eg[li][e].rearrange("(kc p) f -> p kc f", p=128), owner=We_b[i], w=[We_b[i]])
        P.dma("pool", Weu[i][:], C.w_eu[li][e].rearrange("(kc p) f -> p kc f", p=128), owner=We_b[i], w=[We_b[i]])
        P.dma("pool", Wed[i][:], C.w_ed[li][e].rearrange("(fc p) d -> p fc d", p=128), owner=We_b[i], w=[We_b[i]])

    NST = S // TS
    ek = 0
    hi_ = 0
    blk_g = 0
    for sti in range(NST):
        T0 = sti * TS
        for q in range(NB // 4):
            qq = q % 4
            P.dma("sp", yacc[:, q * 4:(q + 1) * 4, :],
                  C.x1_dram[T0 + q * 512:T0 + (q + 1) * 512, :].rearrange("(b p) d -> p b d", p=128),
                  owner=yld_b[qq], r=[C.x1d_b], w=[y_b[q * 4 + j] for j in range(4)])
        if sti == 0:
            load_expert(ek)
        nstate = {}

        def n0(blk):
            sq = ssr[blk % 3]
            sq_b = ssr_b[blk % 3]
            act(P, junk[:], yacc[:, blk, :], AF.Square, r=[y_b[blk]], w=[junk_b, sq_b], accum_out=sq[:, 0:1])
            act(P, sq[:, 1:2], sq[:, 0:1], AF.Ln, r=[sq_b, C.cst_b], w=[sq_b], scale=1.0 / D, bias=C.eps_col[:, 0:1])
            act(P, sq[:, 2:3], sq[:, 1:2], AF.Exp, r=[sq_b], w=[sq_b], scale=-0.5)

        def n1(blk):
            sq = ssr[blk % 3]
            sq_b = ssr_b[blk % 3]
            x_ = xs[blk % 2]
            x_b = xs_b[blk % 2]
            ts(P, "dve", x_[:], yacc[:, blk, :], sq[:, 2:3], ALU.mult, r=[y_b[blk], sq_b], w=[x_b])
            tp = [ring.next(), ring.next()]
            nstate[blk] = tp
            for h in range(2):
                for j in range(4):
                    kc = h * 4 + j
                    tr(P, tp[h][0][:, j * 128:(j + 1) * 128], x_[:, kc * 128:(kc + 1) * 128], C.ident[:],
                       r=[x_b, C.ident_b], w=[tp[h][1]])

        def n2(blk):
            tp = nstate.pop(blk)
            f = hfT[blk % 2]
            f_b = hf_b[blk % 2]
            for h in range(2):
                src = tp[h][0][:].rearrange("p (j t) -> p j t", j=4)
                tt(P, "dve", hmT[:, h * 4:(h + 1) * 4, blk * 128:(blk + 1) * 128], src,
                   C.gbT[:, h * 4:(h + 1) * 4, :], ALU.mult, r=[tp[h][1], C.gb_b], w=[hm_b[blk]])
                tt(P, "dve", f[:, h * 4:(h + 1) * 4, :], src, C.gbT[:, h * 4:(h + 1) * 4, :], ALU.mult,
                   r=[tp[h][1], C.gb_b], w=[f_b])

        def router_mm(blk):
            f = hfT[blk % 2]
            f_b = hf_b[blk % 2]
            pr, pr_b = ring.next()
            nstate[("r", blk)] = (pr, pr_b)
            for kc in range(NKC):
                mm(P, pr[:, 0:20], f[:, kc, :], Wr[:, kc, :], kc == 0, kc == NKC - 1, r=[f_b, Wr_b], w=[pr_b])

        def router(blk):
            pr, pr_b = nstate.pop(("r", blk))
            L = rt[:, 0:20]
            tt(P, "dve", L, pr[:, 0:20], br[:], ALU.add, r=[pr_b, br_b], w=[rt_b])
            m = rt[:, 20:21]
            negm = rt[:, 21:22]
            oh = rt[:, 24:28]
            ex = rt[:, 28:32]
            se = rt[:, 22:23]
            p1 = rt[:, 23:24]
            sel = rt[:, 32:36]
            mk1 = rt[:, 36:40]
            s2 = rt[:, 40:44]
            mk2 = rt[:, 44:48]
            m1 = rt[:, 48:49]
            m2 = rt[:, 49:50]
            dd_ = rt[:, 50:51]
            ed = rt[:, 51:52]
            e1 = rt[:, 52:53]
            a1 = rt[:, 53:54]
            a2 = rt[:, 54:55]
            g4 = rt[:, 56:60]
            R_ = dict(r=[rt_b], w=[rt_b])
            P.op("dve", lambda: nc.vector.tensor_reduce(out=m, in_=rt[:, 0:4], axis=AX.X, op=ALU.max), **R_)
            ts(P, "dve", oh, rt[:, 0:4], m, ALU.is_ge, **R_)
            ts(P, "dve", negm, m, -1.0, ALU.mult, **R_)
            act(P, ex, rt[:, 0:4], AF.Exp, r=[rt_b], w=[rt_b], bias=negm, accum_out=se)
            P.op("dve", lambda: nc.vector.reciprocal(out=p1, in_=se), **R_)
            ts(P, "dve", sel, rt[:, 4:8], oh[:, 0:1], ALU.mult, **R_)
            for g in range(1, 4):
                stt(P, "dve", sel, rt[:, 4 + 4 * g:8 + 4 * g], oh[:, g:g + 1], sel, ALU.mult, ALU.add, **R_)
            P.op("dve", lambda: nc.vector.tensor_reduce(out=m1, in_=sel, axis=AX.X, op=ALU.max), **R_)
            ts(P, "dve", mk1, sel, m1, ALU.is_ge, **R_)
            stt(P, "dve", s2, mk1, -1e30, sel, ALU.mult, ALU.add, **R_)
            P.op("dve", lambda: nc.vector.tensor_reduce(out=m2, in_=s2, axis=AX.X, op=ALU.max), **R_)
            ts(P, "dve", mk2, s2, m2, ALU.is_ge, **R_)
            tt(P, "dve", dd_, m2, m1, ALU.subtract, **R_)
            act(P, ed, dd_, AF.Exp, r=[rt_b], w=[rt_b])
            ts(P, "dve", e1, ed, 1.0, ALU.add, **R_)
            P.op("dve", lambda: nc.vector.reciprocal(out=e1, in_=e1), **R_)
            tt(P, "dve", a1, e1, p1, ALU.mult, **R_)
            tt(P, "dve", a2, ed, a1, ALU.mult, **R_)
            ts(P, "dve", g4, mk1, a1, ALU.mult, **R_)
            stt(P, "dve", g4, mk2, a2, g4, ALU.mult, ALU.add, **R_)
            for g in range(4):
                ts(P, "dve", gate[:, blk, 4 * g:4 * g + 4], g4, oh[:, g:g + 1], ALU.mult, r=[rt_b], w=[gate_b[blk]])

        units = [(e, t5) for e in range(NEXP) for t5 in range(NT5)]
        wslot = {}

        def gu(u):
            nonlocal ek
            e, t5 = units[u]
            if t5 == 0:
                wslot[e] = ek % NW
                ek += 1
            i = wslot[e]
            hd = hid[u % 2]
            hd_b = hid_b[u % 2]
            for fch in range(2):
                pg, pg_b = ring.next()
                for kc in range(NKC):
                    mm(P, pg[:], Weg[i][:, kc, fch * 128:(fch + 1) * 128], hmT[:, kc, t5 * 512:(t5 + 1) * 512],
                       kc == 0, kc == NKC - 1, r=[We_b[i]] + hm_b[t5 * 4:t5 * 4 + 4], w=[pg_b])
                pu, pu_b = ring.next()
                for kc in range(NKC):
                    mm(P, pu[:], Weu[i][:, kc, fch * 128:(fch + 1) * 128], hmT[:, kc, t5 * 512:(t5 + 1) * 512],
                       kc == 0, kc == NKC - 1, r=[We_b[i]] + hm_b[t5 * 4:t5 * 4 + 4], w=[pu_b])
                s_ = sl[(u % 2) * 2 + fch]
                s_b = sl_b[(u % 2) * 2 + fch]
                act(P, s_[:], pg[:], AF.Silu, r=[pg_b], w=[s_b])
                tt(P, "dve", hd[:, fch, :], pu[:], s_[:], ALU.mult, r=[pu_b, s_b], w=[hd_b[fch]])

        def down(u):
            e, t5 = units[u]
            i = wslot[e]
            hd = hid[u % 2]
            hd_b = hid_b[u % 2]
            for half in range(2):
                grp = []
                for bi in (2 * half, 2 * half + 1):
                    for hf in range(2):
                        po, po_b = ring.next()
                        grp.append((bi, hf, po, po_b))
                for fch in range(2):
                    for (bi, hf, po, po_b) in grp:
                        mm(P, po[:], hd[:, fch, bi * 128:(bi + 1) * 128], Wed[i][:, fch, hf * 512:(hf + 1) * 512],
                           fch == 0, fch == 1, r=[hd_b[fch], We_b[i]], w=[po_b])
                for (bi, hf, po, po_b) in grp:
                    blk = t5 * 4 + bi
                    ya = yacc[:, blk, hf * 512:(hf + 1) * 512]
                    stt(P, "dve", ya, po[:], gate[:, blk, e:e + 1], ya, ALU.mult, ALU.add,
                        r=[po_b, gate_b[blk], y_b[blk]], w=[y_b[blk]])

        def unit(u):
            gu(u)
            down(u)
            if units[u][1] == 0 and not (sti == NST - 1 and units[u][0] == NEXP - 1):
                load_expert(ek)

        n0(0)
        for step in range(NB + 2):
            if step + 1 < NB:
                n0(step + 1)
            if step >= 1 and step - 1 < NB:
                router_mm(step - 1)
            if step < NB:
                n1(step)
            if step >= 1 and step - 1 < NB:
                router(step - 1)
            if step < NB:
                n2(step)
            if step >= 4 and step % 4 == 0 and step // 4 - 1 < NT5:
                unit(step // 4 - 1)
        for u in range(NT5, len(units)):
            unit(u)

        def ple1(blk):
            tok = T0 + blk * 128
            i2 = blk % 2
            P.dma("sp", pin[i2][:], C.p_in[li][tok:tok + 128, :], owner=pin_b[i2], w=[pin_b[i2]])
            t0, t0_b = ring.next()
            t1, t1_b = ring.next()
            norm_T(P, C, yacc[:, blk, :], y_b[blk], gbT2, hpT[i2], hp_b[i2], 0, junk, junk_b, ss, ss_b,
                   xs[i2], xs_b[i2], [t0, t1], [t0_b, t1_b], gb_b=gb2_b)
            pp, pp_b = ring.next()
            for c in range(2):
                tr(P, pp[:, c * 128:(c + 1) * 128], pin[i2][:, c * 128:(c + 1) * 128], C.ident[:],
                   r=[pin_b[i2], C.ident_b], w=[pp_b])
            cp(P, "act", pT[i2][:], pp[:, 0:256].rearrange("p (c t) -> p c t", c=2), r=[pp_b], w=[pT_b[i2]])

        def ple2(blk):
            i2 = blk % 2
            for hf in range(2):
                p1_, p1_b = ring.next()
                for kc in range(NKC):
                    mm(P, p1_[:], hpT[i2][:, kc, :], Wpg[:, kc, hf * 512:(hf + 1) * 512], kc == 0, kc == NKC - 1,
                       r=[hp_b[i2], Wpg_b], w=[p1_b])
                p2_, p2_b = ring.next()
                for c in range(2):
                    mm(P, p2_[:], pT[i2][:, c, :], Wpi[:, c, hf * 512:(hf + 1) * 512], c == 0, c == 1,
                       r=[pT_b[i2], Wpi_b], w=[p2_b])
                sg_ = sg[hf]
                sgb = sg_b[hf]
                act(P, sg_[:], p1_[:], AF.Sigmoid, r=[p1_b], w=[sgb])
                tt(P, "dve", sg_[:], p2_[:], sg_[:], ALU.mult, r=[p2_b, sgb], w=[sgb])
                ya = yacc[:, blk, hf * 512:(hf + 1) * 512]
                tt(P, "pool", ya, ya, sg_[:], ALU.add, r=[sgb, y_b[blk]], w=[y_b[blk]])

        def ple3(blk):
            tok = T0 + blk * 128
            i2 = blk % 2
            if not last:
                P.dma("sp", C.x_dram[tok:tok + 128, :], yacc[:, blk, :], owner=st_b[blk % 4], r=[y_b[blk]],
                      w=[C.xd_b[0]])
            else:
                o_ = ob_[i2]
                o_b = ob_b[i2]
                act(P, junk2[:], yacc[:, blk, :], AF.Square, r=[y_b[blk]], w=[junk2_b, ss2_b], accum_out=ss2[:, 0:1])
                act(P, ss2[:, 1:2], ss2[:, 0:1], AF.Ln, r=[ss2_b, C.cst_b], w=[ss2_b], scale=1.0 / D,
                    bias=C.eps_col[:, 0:1])
                act(P, ss2[:, 2:3], ss2[:, 1:2], AF.Exp, r=[ss2_b], w=[ss2_b], scale=-0.5)
                stt(P, "dve", o_[:], yacc[:, blk, :], ss2[:, 2:3], gfin[:], ALU.mult, ALU.mult,
                    r=[y_b[blk], ss2_b, gfin_b], w=[o_b])
                P.dma("sp", C.out[tok:tok + 128, :], o_[:], owner=o_b, r=[o_b], w=[C.out_b])

        PLE_LAGS = ((0, ple1), (0, ple2), (0, ple3)) if not PLE_PIPE else ((2, ple3), (1, ple2), (0, ple1))
        for step in range(NB + 2):
            for lag, fn in PLE_LAGS:
                b_ = step - lag
                if 0 <= b_ < NB:
                    fn(b_)
        P.barrier()
    es.close()
    P.release(bufs)


def build_program(stages=("A", "Bs", "Bd", "C", "D"), debug=(), nlayers=DEPTH):
    nc = bass.Bass("TRN2", target_bir_lowering=False)
    P = Prog(nc)
    C = Ctx()
    ext = lambda name, shape, dt: nc.dram_tensor(name, shape, dt, kind="ExternalInput").ap()
    C.x_in = ext("x", [S, D], F32)
    C.p_in = ext("p", [DEPTH, S, PLE], F32)
    C.positions = ext("positions", [S], I32)
    C.norm_mix = ext("norm_mix", [DEPTH, D], F32)
    C.w_in = ext("w_in", [DEPTH, D, INW], F32)
    C.w_gate = ext("w_gate", [DEPTH, D, 3 * D], F32)
    C.b_gate = ext("b_gate", [DEPTH, 3 * D], F32)
    C.w_pool = ext("w_pool", [DEPTH, 4, 128, 128], F32)
    C.pool_scale = ext("pool_scale", [DEPTH, 512], F32)
    C.w_up_a = ext("w_up_a", [DEPTH, 256, D], F32)
    C.w_up_b = ext("w_up_b", [DEPTH, 512, D], F32)
    C.w_up_c = ext("w_up_c", [DEPTH, 512, D], F32)
    C.w_out = ext("w_out", [DEPTH, D, D], F32)
    C.norm_moe = ext("norm_moe", [DEPTH, D], F32)
    C.w_rg = ext("w_router_grp", [DEPTH, D, 4], F32)
    C.b_rg = ext("b_router_grp", [DEPTH, 4], F32)
    C.w_re = ext("w_router_exp", [DEPTH, 4, D, 4], F32)
    C.b_re = ext("b_router_exp", [DEPTH, 16], F32)
    C.w_eg = ext("w_exp_gate", [DEPTH, NEXP, D, EH], F32)
    C.w_eu = ext("w_exp_up", [DEPTH, NEXP, D, EH], F32)
    C.w_ed = ext("w_exp_down", [DEPTH, NEXP, EH, D], F32)
    C.norm_ple = ext("norm_ple", [DEPTH, D], F32)
    C.w_pi = ext("w_ple_in", [DEPTH, PLE, D], F32)
    C.w_pg = ext("w_ple_gate", [DEPTH, D, D], F32)
    C.norm_final = ext("norm_final", [D], F32)
    C.ident_in = ext("c_ident", [128, 128], F32)
    C.invf_in = ext("c_invf", [128, 8], F32)
    C.rcnt_in = ext("c_rcnt", [4, 16], F32)
    C.tri_in = ext("c_tri", [128, 128], F32)
    C.mband_in = ext("c_mband", [128, 256], F32)
    C.out = nc.dram_tensor("out", [S, D], F32, kind="ExternalOutput").ap()
    C.out_b = P.buf("out")

    dram = lambda name, shape, dt: nc.dram_tensor(name, shape, dt).ap()
    C.x_dram = dram("x_scr", [S, D], F32)
    C.xd_b = [P.buf("xd%d" % i) for i in range(NTT)]
    C.x_dram_b = C.xd_b[0]
    C.QTd = dram("QTd", [DILW, S], BF16); C.QTd_b = P.buf("QTd")
    C.KTd = dram("KTd", [DILW, S], BF16); C.KTd_b = P.buf("KTd")
    C.Vd = dram("Vd", [S, DILW], BF16); C.Vd_b = P.buf("Vd")
    C.QTs = dram("QTs", [SBW, S], BF16); C.QTs_b = P.buf("QTs")
    C.KTs = dram("KTs", [SBW, S], BF16); C.KTs_b = P.buf("KTs")
    C.Vs = dram("Vs", [S, SBW], BF16); C.Vs_b = P.buf("Vs")
    C.YCT = dram("YCT", [512, S], BF16); C.YCT_b = P.buf("YCT")
    C.OAT = dram("OAT", [256, S], BF16); C.OAT_b = P.buf("OAT")
    C.OBT = dram("OBT", [512, S], BF16); C.OBT_b = P.buf("OBT")
    C.HT = dram("HT", [D, S], BF16); C.HT_b = P.buf("HT")
    C.x1_dram = dram("x1_scr", [S, D], F32); C.x1d_b = P.buf("x1d"); C.x1_dram_b = C.x1d_b

    es = contextlib.ExitStack()
    sb = lambda name, shape, dt: es.enter_context(nc.sbuf_tensor(name, shape, dt))
    C.ident = sb("c_ident_sb", [128, 128], F32); C.ident_b = P.buf("ident", True)
    C.invf = sb("c_invf_sb", [128, 8], F32)
    C.sgn = C.invf[:, 1:2]
    C.pi_col = C.invf[:, 2:3]
    C.eps_col = C.invf[:, 3:4]
    C.one_col = C.invf[:, 4:5]
    C.cst_b = P.buf("cst", True)
    C.ones_f = sb("c_ones_f", [128, 512], F32); C.ones_b = P.buf("ones")
    C.gcol = sb("c_gcol", [128, 8], F32); C.gcol_b = P.buf("gcol", True)
    C.gbT = sb("c_gbT", [128, NKC, 128], F32); C.gb_b = P.buf("gbT")
    C.rcnt16 = sb("c_rc16", [128, 4, 16], F32)
    C.rcnt_b = P.buf("rcnt", True)

    P.dma("sp", C.ident[:], C.ident_in[:, :], owner=C.ident_b, w=[C.ident_b])
    P.dma("sp", C.invf[:], C.invf_in[:, :], owner=C.cst_b, w=[C.cst_b])
    P.dma("sp", C.rcnt16[:], C.rcnt_in.partition_broadcast(128), owner=C.rcnt_b, w=[C.rcnt_b])
    P.op("dve", lambda: nc.vector.memset(C.ones_f[:], 1.0), w=[C.ones_b])
    for li in range(DEPTH):
        if "A" in stages:
            phase_A(P, C, li)
        if "Bs" in stages:
            phase_B_sb(P, C, li)
        if "Bd" in stages:
            phase_B_dil(P, C, li)
        if "C" in stages:
            phase_C(P, C, li)
        if "D" in stages:
            phase_D(P, C, li)
        if li + 1 >= nlayers:
            break

    dbg_b = P.buf("dbg", True)
    for name in debug:
        src = getattr(C, name)
        o = nc.dram_tensor("dbg_" + name, list(src.shape), src.dtype, kind="ExternalOutput").ap()
        P.dma("sp", o, src, owner=dbg_b, r=[getattr(C, name + "_b")], w=[dbg_b])
    P.barrier()
    es.close()
    return nc, P


def host_consts():
    ident = np.eye(128, dtype=np.float32)
    invf = np.zeros((128, 8), np.float32)
    p = np.arange(128)
    half = 32
    invf[:, 0] = (10000.0 ** (-(p % 32).astype(np.float32) / np.float32(half))).astype(np.float32)
    invf[:, 1] = np.where((p % 64) < 32, -1.0, 1.0)
    invf[:, 2] = np.pi
    invf[:, 3] = EPS
    invf[:, 4] = 1.0
    t = np.arange(16)
    rcnt = np.stack([1.0 / np.minimum(t + 1, w) for w in POOLW]).astype(np.float32)
    tri = (np.arange(128)[None, :] < np.arange(128)[:, None]).astype(np.float32)
    kk = np.arange(128)[:, None]
    qq = np.arange(128)[None, :]
    mband = np.concatenate([(kk <= qq), (kk >= qq)], axis=1).astype(np.float32)
    return {"c_ident": ident, "c_invf": invf, "c_rcnt": rcnt, "c_tri": tri, "c_mband": mband}


def make_in_maps(inputs):
    consts = host_consts()
    maps = []
    for c in range(8):
        b = c % 4
        m = dict(consts)
        m["x"] = np.ascontiguousarray(inputs["x"][b])
        m["p"] = np.ascontiguousarray(inputs["p"][:, b])
        m["positions"] = np.ascontiguousarray(inputs["positions"][b]).astype(np.int32)
        for k in ("norm_mix", "w_in", "w_gate", "b_gate", "w_pool", "pool_scale", "w_up_a", "w_up_b", "w_up_c",
                  "w_out", "norm_moe", "w_router_grp", "b_router_grp", "w_router_exp", "w_exp_gate", "w_exp_up",
                  "w_exp_down", "norm_ple", "w_ple_in", "w_ple_gate", "norm_final"):
            m[k] = np.ascontiguousarray(inputs[k], dtype=np.float32)
        m["b_router_exp"] = np.ascontiguousarray(inputs["b_router_exp"], dtype=np.float32).reshape(DEPTH, 16)
        maps.append(m)
    return maps


def kernel(**inputs):
    inputs = {k: np.asarray(v) for k, v in inputs.items()}
    nc, _ = build_program()
    res = run_bass_kernel_spmd(nc, make_in_maps(inputs), core_ids=list(range(8)))
    out = np.stack([res.results[b]["out"] for b in range(4)], axis=0)
    return out.astype(np.float32)
```

```python
import contextlib
import numpy as np
import concourse.bass as bass
import concourse.mybir as mybir
from concourse.bass_utils import run_bass_kernel_spmd

F32, BF16, I32 = mybir.dt.float32, mybir.dt.bfloat16, mybir.dt.int32
AF = mybir.ActivationFunctionType
ALU = mybir.AluOpType
AX = mybir.AxisListType

D = 1024
S = 4096
DEPTH = 2
NKC = 8
TT = 512
NTT = S // TT
INW = 4352
DILW = 768
SBW = 512
Q_D, K_D, V_D = 0, 768, 1536
Q_S, K_S, V_S = 2304, 2816, 3328
U_C = 3840
DIL = ((128, 1), (512, 4), (2048, 16))
POOLW = (2, 4, 8, 16)
NEXP = 16
EH = 256
PLE = 256
EPS = 1e-6
TS = 2048
SAME_SYNC = True
PLE_PIPE = True
EXPERT_PIPE = False


class DSem:
    def __init__(self, sem):
        self.sem = sem
        self.cnt = 0


class Buf:
    __slots__ = ("name", "w", "r", "ds")

    def __init__(self, name):
        self.name = name
        self.w = {}
        self.r = {}
        self.ds = None


class Prog:
    def __init__(self, nc):
        self.nc = nc
        self.eng = {"pe": nc.tensor, "act": nc.scalar, "dve": nc.vector, "pool": nc.gpsimd, "sp": nc.sync}
        self.sem = {k: nc.alloc_semaphore("cs_" + k) for k in self.eng}
        self.cnt = {k: 0 for k in self.eng}
        self.seen = {k: {} for k in self.eng}
        self.dsems = []
        self.ds_by_id = {}
        self.free_ds = []
        self.n_ins = 0

    def _get_ds(self):
        if self.free_ds:
            return self.free_ds.pop()
        ds = DSem(self.nc.alloc_semaphore("ds_%d" % len(self.dsems)))
        self.dsems.append(ds)
        self.ds_by_id[id(ds.sem)] = ds
        return ds

    def buf(self, name, dma=False):
        b = Buf(name)
        if dma:
            b.ds = self._get_ds()
        return b

    def release(self, bufs):
        for b in bufs:
            if b.ds is not None:
                self.free_ds.append(b.ds)
                b.ds = None

    def _wait_tok(self, e, k, sem, val):
        ds = self.ds_by_id.get(k)
        if ds is not None:
            val = max(val, ds.cnt)
        seen = self.seen[e]
        if seen.get(k, 0) < val:
            self.eng[e].wait_ge(sem, val)
            seen[k] = val

    def _deps(self, e, r, w):
        own = id(self.sem[e])
        for b in r:
            for k, (sem, val) in b.w.items():
                if k == own and (e == "pe" or not SAME_SYNC):
                    continue
                self._wait_tok(e, k, sem, val)
        for b in w:
            for k, (sem, val) in b.w.items():
                if k == own:
                    continue
                self._wait_tok(e, k, sem, val)
            for k, (sem, val) in b.r.items():
                if k == own:
                    continue
                self._wait_tok(e, k, sem, val)

    def op(self, e, fn, r=(), w=()):
        self._deps(e, r, w)
        ins = fn()
        self.n_ins += 1
        self.cnt[e] += 1
        sem = self.sem[e]
        ins.then_inc(sem, 1)
        tok = (sem, self.cnt[e])
        k = id(sem)
        for b in r:
            b.r[k] = tok
        for b in w:
            b.w[k] = tok
        return ins

    def dma(self, q, out, in_, owner, r=(), w=(), slow=False):
        self._deps(q, r, w)
        if slow:
            ins = self.eng[q].dma_start(out=out, in_=in_, allow_slow_non_contiguous=True)
        else:
            ins = self.eng[q].dma_start(out=out, in_=in_)
        self.n_ins += 1
        ds = owner.ds
        ds.cnt += 16
        ins.then_inc(ds.sem, 16)
        tok = (ds.sem, ds.cnt)
        k = id(ds.sem)
        for b in r:
            b.r[k] = tok
        for b in w:
            b.w[k] = tok
        return ins

    def barrier(self):
        for e in self.eng:
            for e2 in self.eng:
                if e2 != e and self.cnt[e2] > 0:
                    self._wait_tok(e, id(self.sem[e2]), self.sem[e2], self.cnt[e2])
            for ds in self.dsems:
                if ds.cnt > 0:
                    self._wait_tok(e, id(ds.sem), ds.sem, ds.cnt)


class Ctx:
    pass


def act(P, out, in_, func, r, w, bias=None, scale=None, accum_out=None):
    kw = {}
    if bias is not None:
        kw["bias"] = bias
    if scale is not None:
        kw["scale"] = scale
    if accum_out is not None:
        kw["accum_out"] = accum_out
    return P.op("act", lambda: P.nc.scalar.activation(out=out, in_=in_, func=func, **kw), r=r, w=w)


def veng(P, e):
    return P.nc.vector if e == "dve" else P.nc.gpsimd


def tt(P, e, out, in0, in1, op, r, w):
    return P.op(e, lambda: veng(P, e).tensor_tensor(out=out, in0=in0, in1=in1, op=op), r=r, w=w)


def ts(P, e, out, in0, s1, op0, r, w, s2=None, op1=None):
    if op1 is None:
        return P.op(e, lambda: veng(P, e).tensor_scalar(out=out, in0=in0, scalar1=s1, scalar2=None, op0=op0), r=r, w=w)
    return P.op(e, lambda: veng(P, e).tensor_scalar(out=out, in0=in0, scalar1=s1, scalar2=s2, op0=op0, op1=op1), r=r, w=w)


def stt(P, e, out, in0, scalar, in1, op0, op1, r, w):
    return P.op(e, lambda: veng(P, e).scalar_tensor_tensor(out=out, in0=in0, scalar=scalar, in1=in1, op0=op0, op1=op1), r=r, w=w)


def cp(P, e, out, in_, r, w):
    if e == "act":
        return P.op("act", lambda: P.nc.scalar.copy(out=out, in_=in_), r=r, w=w)
    return P.op(e, lambda: veng(P, e).tensor_copy(out=out, in_=in_), r=r, w=w)


def mm(P, out, lhsT, rhs, start, stop, r, w):
    return P.op("pe", lambda: P.nc.tensor.matmul(out, lhsT=lhsT, rhs=rhs, start=start, stop=stop), r=r, w=w)


def tr(P, out, in_, ident, r, w):
    return P.op("pe", lambda: P.nc.tensor.transpose(out, in_, ident), r=r, w=w)


def norm_T(P, C, xblk, xb, gbT, outT, outT_b, col0, junk, junk_b, ss, ss_b, xs, xs_b, tps, tps_b,
           outF=None, outF_b=None, gb_b=None):
    nc = P.nc
    gb_b = gb_b if gb_b is not None else C.gb_b
    act(P, junk[:], xblk, AF.Square, r=[xb], w=[junk_b, ss_b], accum_out=ss[:, 0:1])
    act(P, ss[:, 1:2], ss[:, 0:1], AF.Ln, r=[ss_b, C.cst_b], w=[ss_b], scale=1.0 / D, bias=C.eps_col[:, 0:1])
    act(P, ss[:, 2:3], ss[:, 1:2], AF.Exp, r=[ss_b], w=[ss_b], scale=-0.5)
    ts(P, "dve", xs[:], xblk, ss[:, 2:3], ALU.mult, r=[xb, ss_b], w=[xs_b])
    for h in range(2):
        for j in range(4):
            kc = h * 4 + j
            tr(P, tps[h][:, j * 128:(j + 1) * 128], xs[:, kc * 128:(kc + 1) * 128], C.ident[:],
               r=[xs_b, C.ident_b], w=[tps_b[h]])
        src = tps[h][:].rearrange("p (j t) -> p j t", j=4)
        tt(P, "dve", outT[:, h * 4:(h + 1) * 4, col0:col0 + 128], src, gbT[:, h * 4:(h + 1) * 4, :], ALU.mult,
           r=[tps_b[h], gb_b], w=[outT_b])
        if outF is not None:
            tt(P, "dve", outF[:, h * 4:(h + 1) * 4, :], src, gbT[:, h * 4:(h + 1) * 4, :], ALU.mult,
               r=[tps_b[h], gb_b], w=[outF_b])


def load_gain(P, C, gvec_dram, gbT, es_name, gb_b=None):
    nc = P.nc
    P.dma("sp", C.gcol[:], gvec_dram.rearrange("(c p) -> p c", p=128), owner=C.gcol_b, w=[C.gcol_b], slow=True)
    gb_b = gb_b if gb_b is not None else C.gb_b
    for kc in range(NKC):
        ts(P, "pool", gbT[:, kc, :], C.ones_f[:, 0:128], C.gcol[:, kc:kc + 1], ALU.mult,
           r=[C.gcol_b, C.ones_b], w=[gb_b])


def phase_A(P, C, li):
    nc = P.nc
    es = contextlib.ExitStack()
    sb = lambda name, shape, dt: es.enter_context(nc.sbuf_tensor(name + "_L%d" % li, shape, dt))
    ps = lambda name, shape, dt: es.enter_context(nc.psum_tensor(name + "_L%d" % li, shape, dt))
    bufs = []

    def B(name, dma=False):
        b = P.buf(name, dma)
        bufs.append(b)
        return b

    Win = sb("A_win", [128, NKC, INW], BF16)
    Win_b = [B("A_win%d" % kc, True) for kc in range(NKC)]
    Wsw = sb("A_wsw", [128, NKC, 2 * DILW], BF16)
    Wsw_b = B("A_wsw")
    Wp = sb("A_wp", [128, 4, 128], BF16)
    Wp_b = B("A_wp", True)
    psc = sb("A_psc", [128, 4], F32)
    psc_b = B("A_psc", True)
    xt = [sb("A_xt0", [128, 4, D], F32)] * 2
    xt_b = [B("A_xt0", True)] * 2
    C.cosT = sb("A_cosT", [128, S], F32)
    C.sinT = sb("A_sinT", [128, S], F32)
    C.rope_b = B("rope")
    hT = [sb("A_hT%d" % i, [128, NKC, TT], BF16) for i in range(2)]
    hT_b = [B("A_hT%d" % i, True) for i in range(2)]
    junk = sb("A_junk", [128, D], BF16)
    junk_b = B("A_junk")
    ssq = [sb("A_ssq%d" % i, [128, 4], F32) for i in range(4)]
    ssq_b = [B("A_ssq%d" % i) for i in range(4)]
    xs = [sb("A_xs%d" % i, [128, D], F32) for i in range(2)]
    xs_b = [B("A_xs%d" % i) for i in range(2)]
    tps = [ps("A_tp%d" % i, [128, 512], F32) for i in range(4)]
    tps_b = [B("A_tp%d" % i) for i in range(4)]
    pj = [ps("A_pj%d" % i, [128, 512], F32) for i in range(4)]
    pj_b = [B("A_pj%d" % i) for i in range(4)]
    t1 = [sb("A_t1_%d" % i, [128, TT], F32) for i in range(2)]
    t1_b = [B("A_t1_%d" % i) for i in range(2)]
    t2 = [sb("A_t2_%d" % i, [128, TT], F32) for i in range(2)]
    t2_b = [B("A_t2_%d" % i) for i in range(2)]
    NST = 4
    stg = [sb("A_stg%d" % i, [128, TT], BF16) for i in range(NST)]
    stg_b = [B("A_stg%d" % i, True) for i in range(NST)]
    vst = [sb("A_vst%d" % i, [128, DILW + SBW], BF16) for i in range(2)]
    vst_b = [B("A_vst%d" % i, True) for i in range(2)]
    ub = [sb("A_ub%d" % g, [128, 16 + TT], F32) for g in range(4)]
    ub_b = [B("A_ub%d" % g) for g in range(4)]
    pw = [sb("A_pw%d" % i, [128, 16 + TT], F32) for i in range(2)]
    pw_b = [B("A_pw%d" % i) for i in range(2)]
    pl = [sb("A_pl%d" % i, [128, TT], BF16) for i in range(2)]
    pl_b = [B("A_pl%d" % i) for i in range(2)]

    build_rope(P, C, xt[0][:].rearrange("p b d -> p (b d)"), xt_b[0])
    w_in = C.w_in[li]
    for kc in range(NKC):
        P.dma("pool", Win[:, kc, :], w_in[kc * 128:(kc + 1) * 128, :], owner=Win_b[kc], w=[Win_b[kc]])
    P.dma("pool", Wp[:], C.w_pool[li].rearrange("g c d -> c g d"), owner=Wp_b, w=[Wp_b])
    P.dma("sp", psc[:], C.pool_scale[li].rearrange("(g p) -> p g", p=128), owner=psc_b, w=[psc_b], slow=True)
    load_gain(P, C, C.norm_mix[li], C.gbT, "A")
    for kc in range(NKC):
        src = Win[:, kc, 0:2 * DILW].rearrange("p (h two c) -> p h two c", two=2, c=32)
        dst = Wsw[:, kc, :].rearrange("p (h two c) -> p h two c", two=2, c=32)
        e = "dve" if kc % 2 == 0 else "pool"
        cp(P, e, dst[:, :, 0, :], src[:, :, 1, :], r=[Win_b[kc]], w=[Wsw_b])
        cp(P, e, dst[:, :, 1, :], src[:, :, 0, :], r=[Win_b[kc]], w=[Wsw_b])
    for g in range(4):
        P.op("pool", lambda g=g: nc.gpsimd.memset(ub[g][:, 0:16], 0.0), w=[ub_b[g]])

    xd = C.x_in if li == 0 else C.x_dram
    sti = 0
    pji = 0

    def load_x(t_):
        p_ = t_ % 2
        P.dma("sp", xt[p_][:], xd[t_ * TT:(t_ + 1) * TT, :].rearrange("(b p) d -> p b d", p=128),
              owner=xt_b[p_], r=[C.xd_b[t_]], w=[xt_b[p_]])

    def nA0(t_):
        p_ = t_ % 2
        for bi in range(4):
            q, q_b = ssq[bi], ssq_b[bi]
            act(P, junk[:], xt[p_][:, bi, :], AF.Square, r=[xt_b[p_]], w=[junk_b, q_b], accum_out=q[:, 0:1])
            act(P, q[:, 1:2], q[:, 0:1], AF.Ln, r=[q_b, C.cst_b], w=[q_b], scale=1.0 / D, bias=C.eps_col[:, 0:1])
            act(P, q[:, 2:3], q[:, 1:2], AF.Exp, r=[q_b], w=[q_b], scale=-0.5)

    def nA1(t_, bi):
        p_ = t_ % 2
        ts(P, "dve", xs[bi % 2][:], xt[p_][:, bi, :], ssq[bi][:, 2:3], ALU.mult, r=[xt_b[p_], ssq_b[bi]],
           w=[xs_b[bi % 2]])

    def nA2(t_, bi):
        for h_ in range(2):
            tp, tp_b = tps[(bi % 2) * 2 + h_], tps_b[(bi % 2) * 2 + h_]
            for j in range(4):
                kc = h_ * 4 + j
                tr(P, tp[:, j * 128:(j + 1) * 128], xs[bi % 2][:, kc * 128:(kc + 1) * 128], C.ident[:],
                   r=[xs_b[bi % 2], C.ident_b], w=[tp_b])

    def nA3(t_, bi):
        p_ = t_ % 2
        for h_ in range(2):
            tp, tp_b = tps[(bi % 2) * 2 + h_], tps_b[(bi % 2) * 2 + h_]
            tt(P, "dve", hT[p_][:, h_ * 4:(h_ + 1) * 4, bi * 128:(bi + 1) * 128],
               tp[:].rearrange("p (j t) -> p j t", j=4), C.gbT[:, h_ * 4:(h_ + 1) * 4, :], ALU.mult,
               r=[tp_b, C.gb_b], w=[hT_b[p_]])

    def norm_body(t_):
        nA1(t_, 0); nA1(t_, 1); nA2(t_, 0); nA3(t_, 0); nA1(t_, 2); nA2(t_, 1); nA3(t_, 1); nA1(t_, 3)
        nA2(t_, 2); nA3(t_, 2); nA2(t_, 3); nA3(t_, 3)
        if t_ + 1 < NTT:
            load_x(t_ + 1)

    load_x(0)
    nA0(0)
    norm_body(0)
    for ti in range(NTT):
        pb = ti % 2
        tok0 = ti * TT
        h = hT[pb]
        hb = hT_b[pb]
        P.dma("sp", C.HT[:, tok0:tok0 + TT].rearrange("(kc p) t -> p kc t", p=128), h[:], owner=hb, r=[hb],
              w=[C.HT_b])

        def proj_fm(col0, W=Win, Wb=None):
            nonlocal pji
            o = pj[pji % 4]
            ob = pj_b[pji % 4]
            pji += 1
            for kc in range(NKC):
                mm(P, o[:], W[:, kc, col0:col0 + 128], h[:, kc, :], kc == 0, kc == NKC - 1,
                   r=[hb, (Wb if Wb is not None else Win_b[kc])], w=[ob])
            return o, ob

        for which, cbase, dst in ((0, Q_D, C.QTd), (1, K_D, C.KTd)):
            if ti + 1 < NTT:
                if which == 1:
                    nA0(ti + 1)
            for ci in range(6):
                if which == 1 and ci == 4 and ti + 1 < NTT:
                    norm_body(ti + 1)
                a, a_b = proj_fm(cbase + ci * 128)
                s_, s_b = proj_fm(cbase + ci * 128, W=Wsw, Wb=Wsw_b)
                i2 = ci % 2
                tt(P, "dve", t1[i2][:], a[:], C.cosT[:, tok0:tok0 + TT], ALU.mult, r=[a_b, C.rope_b], w=[t1_b[i2]])
                tt(P, "dve", t2[i2][:], s_[:], C.sinT[:, tok0:tok0 + TT], ALU.mult, r=[s_b, C.rope_b], w=[t2_b[i2]])
                st = stg[sti % NST]
                st_b = stg_b[sti % NST]
                sti += 1
                tt(P, "pool", st[:], t1[i2][:], t2[i2][:], ALU.add, r=[t1_b[i2], t2_b[i2]], w=[st_b])
                P.dma("sp", dst[ci * 128:(ci + 1) * 128, tok0:tok0 + TT], st[:], owner=st_b, r=[st_b],
                      w=[C.QTd_b if which == 0 else C.KTd_b])
        for which, cbase, dst in ((0, Q_S, C.QTs), (1, K_S, C.KTs)):
            for ci in range(4):
                a, a_b = proj_fm(cbase + ci * 128)
                st = stg[sti % NST]
                st_b = stg_b[sti % NST]
                sti += 1
                cp(P, "act", st[:], a[:], r=[a_b], w=[st_b])
                P.dma("sp", dst[ci * 128:(ci + 1) * 128, tok0:tok0 + TT], st[:], owner=st_b, r=[st_b],
                      w=[C.QTs_b if which == 0 else C.KTs_b])
        pend = []
        for g in range(4):
            a, a_b = proj_fm(U_C + g * 128)
            u = ub[g]
            cp(P, "act", u[:, 16:16 + TT], a[:], r=[a_b], w=[ub_b[g]])
            wdw = POOLW[g]
            cur, cur_b = u, ub_b[g]
            sh = 1
            lvl = 0
            while sh < wdw:
                o = pw[lvl % 2]
                o_b = pw_b[lvl % 2]
                lo = 2 * sh - 1
                tt(P, "pool", o[:, lo:16 + TT], cur[:, lo:16 + TT], cur[:, lo - sh:16 + TT - sh], ALU.add,
                   r=[cur_b], w=[o_b])
                cur, cur_b = o, o_b
                sh *= 2
                lvl += 1
            o = pw[lvl % 2]
            o_b = pw_b[lvl % 2]
            if ti == 0:
                tt(P, "pool", o[:, 16:32], cur[:, 16:32], C.rcnt16[:, g, :], ALU.mult, r=[cur_b, C.rcnt_b], w=[o_b])
                ts(P, "pool", o[:, 32:16 + TT], cur[:, 32:16 + TT], 1.0 / wdw, ALU.mult, r=[cur_b], w=[o_b])
            else:
                ts(P, "pool", o[:, 16:16 + TT], cur[:, 16:16 + TT], 1.0 / wdw, ALU.mult, r=[cur_b], w=[o_b])
            i2 = g % 2
            tt(P, "pool", pl[i2][:], o[:, 16:16 + TT], u[:, 16:16 + TT], ALU.subtract, r=[o_b, ub_b[g]], w=[pl_b[i2]])
            cp(P, "pool", u[:, 0:16], u[:, TT:TT + 16], r=[ub_b[g]], w=[ub_b[g]])

            def p2(g=g, i2=i2, tok0=tok0):
                nonlocal pji, sti
                y = pj[pji % 4]
                y_b = pj_b[pji % 4]
                pji += 1
                mm(P, y[:], Wp[:, g, :], pl[i2][:], True, True, r=[Wp_b, pl_b[i2]], w=[y_b])
                st = stg[sti % NST]
                st_b = stg_b[sti % NST]
                sti += 1
                ts(P, "dve", st[:], y[:], psc[:, g:g + 1], ALU.mult, r=[y_b, psc_b], w=[st_b])
                P.dma("sp", C.YCT[g * 128:(g + 1) * 128, tok0:tok0 + TT], st[:], owner=st_b, r=[st_b],
                      w=[C.YCT_b])

            if pend:
                pend.pop()()
            pend.append(p2)
        for bi in range(4):
            vs = vst[bi % 2]
            vs_b = vst_b[bi % 2]
            for (c0, cw, o0) in ((V_D, 512, 0), (V_D + 512, 256, 512), (V_S, 512, DILW)):
                o = pj[pji % 4]
                ob = pj_b[pji % 4]
                pji += 1
                for kc in range(NKC):
                    mm(P, o[:, 0:cw], h[:, kc, bi * 128:(bi + 1) * 128], Win[:, kc, c0:c0 + cw], kc == 0,
                       kc == NKC - 1, r=[hb, Win_b[kc]], w=[ob])
                cp(P, "act" if o0 != 512 else "dve", vs[:, o0:o0 + cw], o[:, 0:cw], r=[ob], w=[vs_b])
                if pend:
                    pend.pop()()
            t0 = tok0 + bi * 128
            P.dma("sp", C.Vd[t0:t0 + 128, :], vs[:, 0:DILW], owner=vs_b, r=[vs_b], w=[C.Vd_b])
            P.dma("sp", C.Vs[t0:t0 + 128, :], vs[:, DILW:DILW + SBW], owner=vs_b, r=[vs_b], w=[C.Vs_b])
    P.barrier()
    es.close()
    P.release(bufs)


def build_rope(P, C, pi_f32_tile, pi_b):
    nc = P.nc
    pi = pi_f32_tile.bitcast(I32)
    P.dma("sp", pi, C.positions.partition_broadcast(128), owner=pi_b, w=[pi_b])
    cp(P, "dve", C.cosT[:], pi, r=[pi_b], w=[C.rope_b])
    A_ = C.sinT
    T_ = C.cosT
    ts(P, "dve", A_[:], T_[:], C.invf[:, 0:1], ALU.mult, r=[C.rope_b, C.cst_b], w=[C.rope_b])
    ts(P, "dve", T_[:], A_[:], float(1.0 / (2 * np.pi)), ALU.mult, r=[C.rope_b], w=[C.rope_b])
    cp(P, "dve", pi, T_[:], r=[C.rope_b], w=[pi_b])
    cp(P, "dve", T_[:], pi, r=[pi_b], w=[C.rope_b])
    C1 = 6.28125
    C2 = float(2 * np.pi - 6.28125)
    stt(P, "dve", A_[:], T_[:], -C1, A_[:], ALU.mult, ALU.add, r=[C.rope_b], w=[C.rope_b])
    stt(P, "dve", A_[:], T_[:], -C2, A_[:], ALU.mult, ALU.add, r=[C.rope_b], w=[C.rope_b])
    ts(P, "dve", T_[:], A_[:], float(np.pi / 2), ALU.is_gt, r=[C.rope_b], w=[C.rope_b])
    stt(P, "dve", T_[:], T_[:], float(-2 * np.pi), A_[:], ALU.mult, ALU.add, r=[C.rope_b], w=[C.rope_b])
    LIM = 3.1415925
    ts(P, "dve", T_[:], T_[:], float(np.pi / 2), ALU.add, r=[C.rope_b], w=[C.rope_b], s2=LIM, op1=ALU.min)
    ts(P, "dve", T_[:], T_[:], -LIM, ALU.max, r=[C.rope_b], w=[C.rope_b])
    ts(P, "dve", A_[:], A_[:], LIM, ALU.min, r=[C.rope_b], w=[C.rope_b], s2=-LIM, op1=ALU.max)
    act(P, C.cosT[:], T_[:], AF.Sin, r=[C.rope_b], w=[C.rope_b])
    act(P, C.sinT[:], A_[:], AF.Sin, r=[C.rope_b], w=[C.rope_b])
    ts(P, "dve", C.sinT[:], C.sinT[:], C.sgn[:, 0:1], ALU.mult, r=[C.rope_b, C.cst_b], w=[C.rope_b])


def phase_B_sb(P, C, li):
    nc = P.nc
    es = contextlib.ExitStack()
    sb = lambda name, shape, dt: es.enter_context(nc.sbuf_tensor(name + "_L%d" % li, shape, dt))
    ps = lambda name, shape, dt: es.enter_context(nc.psum_tensor(name + "_L%d" % li, shape, dt))
    bufs = []

    def B(name, dma=False):
        b = P.buf(name, dma)
        bufs.append(b)
        return b

    KT = [sb("S_KT%d" % i, [64, S], BF16) for i in range(2)]
    QT = [sb("S_QT%d" % i, [64, S], BF16) for i in range(2)]
    V = [sb("S_V%d" % i, [128, 32, 64], BF16) for i in range(2)]
    hd_b = [B("S_hd%d" % i, True) for i in range(2)]
    sp = [sb("S_sp%d" % i, [128, S], F32) for i in range(2)]
    sp_b = [[B("S_sp%d_%d" % (i, c)) for c in range(8)] for i in range(2)]
    ND = 3
    dd = [sb("S_d%d" % i, [128, S], F32) for i in range(ND)]
    dd_b = [B("S_d%d" % i) for i in range(ND)]
    G = [sb("S_G%d" % i, [128, S], F32) for i in range(2)]
    G_b = [B("S_G%d" % i) for i in range(2)]
    NN = 4
    nt = [sb("S_nt%d" % i, [128, 2], F32) for i in range(NN)]
    nt_b = [B("S_nt%d" % i) for i in range(NN)]
    A = [sb("S_A%d" % i, [128, S], BF16) for i in range(2)]
    A_b = [B("S_A%d" % i) for i in range(2)]
    AT = [sb("S_AT%d" % i, [128, S], BF16) for i in range(2)]
    AT_b = [B("S_AT%d" % i) for i in range(2)]
    NOT = 1
    OT = [sb("S_OT%d" % i, [64, S], BF16) for i in range(NOT)]
    OT_b = [B("S_OT%d" % i, True) for i in range(NOT)]
    NZ = 4
    NE = 3
    et = [sb("S_et%d" % i, [128, 512], F32) for i in range(NE)]
    et_b = [B("S_et%d" % i) for i in range(NE)]
    trif = sb("S_trif", [128, 128], F32)
    trib = sb("S_trib", [128, 128], BF16)
    idb = sb("S_idb", [128, 128], BF16)
    tri_b = B("S_tri", True)
    zps = [ps("S_z%d" % i, [128, 512], F32) for i in range(NZ)]
    zps_b = [B("S_z%d" % i) for i in range(NZ)]
    tpp = [ps("S_tp%d" % i, [128, 512], BF16) for i in range(2)]
    tpp_b = [B("S_tp%d" % i) for i in range(2)]
    ots = [ps("S_o%d" % i, [64, 128], F32) for i in range(2)]
    ots_b = [B("S_o%d" % i) for i in range(2)]

    P.dma("sp", trif[:], C.tri_in[:, :], owner=tri_b, w=[tri_b])
    mneg = trib
    ts(P, "dve", mneg[:], trif[:], 30000.0, ALU.mult, r=[tri_b], w=[tri_b], s2=-30000.0, op1=ALU.add)
    cp(P, "dve", idb[:], C.ident[:], r=[C.ident_b], w=[tri_b])

    def load_head(hd):
        i = hd % 2
        P.dma("sp", KT[i][:], C.KTs[hd * 64:(hd + 1) * 64, :], owner=hd_b[i], r=[C.KTs_b], w=[hd_b[i]])
        P.dma("sp", QT[i][:], C.QTs[hd * 64:(hd + 1) * 64, :], owner=hd_b[i], r=[C.QTs_b], w=[hd_b[i]])
        P.dma("sp", V[i][:], C.Vs[:, hd * 64:(hd + 1) * 64].rearrange("(b p) c -> p b c", p=128), owner=hd_b[i],
              r=[C.Vs_b], w=[hd_b[i]])

    blocks = [(hd, qb) for hd in range(8) for qb in range(32)]
    NBLK = len(blocks)
    cnt = {"z": 0, "t": 0, "o": 0}

    def s1(k):
        hd, qb = blocks[k]
        i = hd % 2
        rb = k % 2
        db = k % ND
        nk = 128 * (qb + 1)
        nkc = (nk + 511) // 512
        pend = None
        for kc in range(nkc):
            w = min(512, nk - kc * 512)
            zi = cnt["z"]
            cnt["z"] += 1
            z = zps[zi % NZ]
            z_b = zps_b[zi % NZ]
            e = et[zi % NE]
            e_b = et_b[zi % NE]
            q_ = QT[i][:, qb * 128:(qb + 1) * 128]
            if kc < nkc - 1:
                mm(P, z[:, :w], q_, KT[i][:, kc * 512:kc * 512 + w], True, True, r=[hd_b[i]], w=[z_b])
            else:
                if w > 128:
                    mm(P, z[:, :w - 128], q_, KT[i][:, kc * 512:kc * 512 + w - 128], True, True, r=[hd_b[i]], w=[z_b])
                mm(P, z[:, w - 128:w], q_, KT[i][:, kc * 512 + w - 128:kc * 512 + w], True, False, r=[hd_b[i]],
                   w=[z_b])
                mm(P, z[:, w - 128:w], idb[:], mneg[:], False, True, r=[tri_b], w=[z_b])
            act(P, e[:, :w], z[:, :w], AF.Exp, r=[z_b], w=[e_b], scale=0.125)

            def tail(kc=kc, w=w, z=z, z_b=z_b, e=e, e_b=e_b):
                act(P, sp[rb][:, kc * 512:kc * 512 + w], e[:, :w], AF.Ln, r=[e_b, C.cst_b], w=[sp_b[rb][kc]],
                    bias=C.one_col[:, 0:1])
                stt(P, "dve", dd[db][:, kc * 512:kc * 512 + w], z[:, :w], 0.125, sp[rb][:, kc * 512:kc * 512 + w],
                    ALU.mult, ALU.subtract, r=[z_b, sp_b[rb][kc]], w=[dd_b[db]])

            if pend is not None:
                pend()
            pend = tail
        if pend is not None:
            pend()

    def s2a(k):
        hd, qb = blocks[k]
        rb = k % 2
        nk = 128 * (qb + 1)
        nkc = (nk + 511) // 512
        g_ = G[k % 2]
        g_b = G_b[k % 2]
        n_ = nt[k % NN]
        n_b = nt_b[k % NN]
        spb = sp_b[rb][:nkc]
        P.op("dve", lambda: nc.vector.tensor_tensor_scan(
            out=g_[:, :nk], data0=sp[rb][:, :nk], data1=sp[rb][:, :nk], initial=0.0, op0=ALU.add, op1=ALU.max),
            r=spb, w=[g_b])
        ts(P, "dve", n_[:, 0:1], g_[:, nk - 1:nk], -1.0, ALU.mult, r=[g_b], w=[n_b])

    def s2b(k):
        hd, qb = blocks[k]
        db = k % ND
        nk = 128 * (qb + 1)
        tt(P, "pool", dd[db][:, :nk], dd[db][:, :nk], G[k % 2][:, :nk], ALU.add, r=[dd_b[db], G_b[k % 2]],
           w=[dd_b[db]])

    def s2c(k):
        hd, qb = blocks[k]
        rb = k % 2
        db = k % ND
        nk = 128 * (qb + 1)
        n_ = nt[k % NN]
        n_b = nt_b[k % NN]
        act(P, A[rb][:, :nk], dd[db][:, :nk], AF.Exp, r=[dd_b[db], n_b], w=[A_b[rb]], bias=n_[:, 0:1])

    def s3(k):
        hd, qb = blocks[k]
        i = hd % 2
        rb = k % 2
        ot = OT[hd % NOT]
        ot_b = OT_b[hd % NOT]
        for kb4 in range(0, qb + 1, 4):
            n4 = min(4, qb + 1 - kb4)
            ti = cnt["t"]
            cnt["t"] += 1
            tp = tpp[ti % 2]
            tp_b = tpp_b[ti % 2]
            for j in range(n4):
                tr(P, tp[:, j * 128:(j + 1) * 128], A[rb][:, (kb4 + j) * 128:(kb4 + j + 1) * 128], idb[:],
                   r=[A_b[rb], tri_b], w=[tp_b])
            cp(P, "dve" if ti % 2 == 0 else "act", AT[rb][:, kb4 * 128:(kb4 + n4) * 128], tp[:, :n4 * 128],
               r=[tp_b], w=[AT_b[rb]])

    def s3b(k):
        hd, qb = blocks[k]
        i = hd % 2
        rb = k % 2
        o = ots[k % 2]
        o_b = ots_b[k % 2]
        for kb in range(qb + 1):
            mm(P, o[:, :], V[i][:, kb, :], AT[rb][:, kb * 128:(kb + 1) * 128], kb == 0, kb == qb,
               r=[hd_b[i], AT_b[rb]], w=[o_b])

    def s4(k):
        hd, qb = blocks[k]
        ot = OT[hd % NOT]
        ot_b = OT_b[hd % NOT]
        o = ots[k % 2]
        o_b = ots_b[k % 2]
        cp(P, "act", ot[:, qb * 128:(qb + 1) * 128], o[:, :], r=[o_b], w=[ot_b])
        if qb == 31:
            P.dma("sp", C.OBT[hd * 64:(hd + 1) * 64, :], ot[:], owner=ot_b, r=[ot_b], w=[C.OBT_b])

    load_head(0)
    for step in range(NBLK + 5):
        for lag, fn in ((4, s3), (3, s2c), (2, s2b), (1, s2a), (0, s1), (4, s3b), (5, s4)):
            k = step - lag
            if 0 <= k < NBLK:
                fn(k)
        if step % 32 == 5 and step // 32 + 1 < 8:
            load_head(step // 32 + 1)
    P.barrier()
    es.close()
    P.release(bufs)


def phase_B_dil(P, C, li):
    nc = P.nc
    es = contextlib.ExitStack()
    sb = lambda name, shape, dt: es.enter_context(nc.sbuf_tensor(name + "_L%d" % li, shape, dt))
    ps = lambda name, shape, dt: es.enter_context(nc.psum_tensor(name + "_L%d" % li, shape, dt))
    bufs = []

    def B(name, dma=False):
        b = P.buf(name, dma)
        bufs.append(b)
        return b

    KT = [sb("L_KT%d" % i, [64, S], BF16) for i in range(2)]
    QT = [sb("L_QT%d" % i, [64, S], BF16) for i in range(2)]
    Vr = [sb("L_V%d" % i, [128, 32, 64], BF16) for i in range(2)]
    hd_b = [B("L_hd%d" % i, True) for i in range(2)]
    NDs = [sb("L_ND%d" % i, [64, 2, S], F32) for i in range(2)]
    NDs_b = [B("L_ND%d" % i) for i in range(2)]
    OA = sb("L_OA", [64, S], BF16)
    OA_b = B("L_OA", True)
    mbf = sb("L_mbf", [128, 256], F32)
    mbb = sb("L_mbb", [128, 256], BF16)
    onb = sb("L_onb", [128, 64], BF16)
    idb = sb("L_idb", [128, 128], BF16)
    mb_b = B("L_mb", True)
    Pm = [sb("L_P%d" % i, [128, 512], BF16) for i in range(3)]
    Pm_b = [B("L_P%d" % i) for i in range(3)]
    stp = [ps("L_st%d" % i, [128, 512], F32) for i in range(3)]
    stp_b = [B("L_st%d" % i) for i in range(3)]
    ndp = [ps("L_nd%d" % i, [64, 512], F32) for i in range(3)]
    ndp_b = [B("L_nd%d" % i) for i in range(3)]

    P.dma("sp", mbf[:], C.mband_in[:, :], owner=mb_b, w=[mb_b])
    ts(P, "dve", mbb[:], mbf[:], 30000.0, ALU.mult, r=[mb_b], w=[mb_b], s2=-30000.0, op1=ALU.add)
    cp(P, "dve", idb[:], C.ident[:], r=[C.ident_b], w=[mb_b])
    P.op("dve", lambda: nc.vector.memset(onb[:], 1.0), w=[mb_b])

    heads = [(j, g) for j in range(4) for g in range(3)]

    def load_head(idx):
        j, g = heads[idx]
        d = DIL[g][1]
        hh = g * 4 + j
        i = idx % 2
        P.dma("sp", KT[i][:], C.KTd[hh * 64:(hh + 1) * 64, :], owner=hd_b[i], r=[C.KTd_b], w=[hd_b[i]])
        P.dma("sp", QT[i][:], C.QTd[hh * 64:(hh + 1) * 64, :], owner=hd_b[i], r=[C.QTd_b], w=[hd_b[i]])
        nbn = 32 // d
        src = C.Vd[:, hh * 64:(hh + 1) * 64].rearrange("(nb i r) c -> r i nb c", i=128, r=d)
        for r in range(d):
            P.dma("sp", Vr[i][:, r * nbn:(r + 1) * nbn, :], src[r], owner=hd_b[i], r=[C.Vd_b], w=[hd_b[i]])

    blocks = []
    first_of = {}
    last_of = {}
    for idx, (j, g) in enumerate(heads):
        d = DIL[g][1]
        nbn = 32 // d
        first_of[idx] = len(blocks)
        for r in range(d):
            for p_ in range(nbn // 2):
                blocks.append((idx, r, p_))
        last_of[idx] = len(blocks) - 1
    NBLK = len(blocks)

    def geom(k):
        idx, r, p_ = blocks[k]
        j, g = heads[idx]
        d = DIL[g][1]
        nbn = 32 // d
        return idx, r, p_, j, g, d, nbn

    def qsl(base, d, n=128):
        return slice(base, base + (n - 1) * d + 1, d) if d > 1 else slice(base, base + n)

    def dA(k):
        idx, r, p_, j, g, d, nbn = geom(k)
        i = idx % 2
        st = stp[k % 3]
        st_b = stp_b[k % 3]
        for b in range(2):
            nb = 2 * p_ + b
            base = nb * 128 * d + r
            sl = qsl(base, d)
            o = b * 256
            mm(P, st[:, o:o + 128], KT[i][:, sl], QT[i][:, sl], True, False, r=[hd_b[i]], w=[st_b])
            mm(P, st[:, o:o + 128], idb[:], mbb[:, 0:128], False, True, r=[mb_b], w=[st_b])
            if nb > 0:
                slp = qsl(base - 128 * d, d)
                mm(P, st[:, o + 128:o + 256], KT[i][:, slp], QT[i][:, sl], True, False, r=[hd_b[i]], w=[st_b])
                mm(P, st[:, o + 128:o + 256], idb[:], mbb[:, 128:256], False, True, r=[mb_b], w=[st_b])

    def dB(k):
        idx, r, p_, j, g, d, nbn = geom(k)
        st, st_b, pm, pm_b = stp[k % 3], stp_b[k % 3], Pm[k % 3], Pm_b[k % 3]
        if p_ == 0:
            act(P, pm[:, 0:128], st[:, 0:128], AF.Exp, r=[st_b], w=[pm_b], scale=0.125)
            act(P, pm[:, 256:512], st[:, 256:512], AF.Exp, r=[st_b], w=[pm_b], scale=0.125)
        else:
            act(P, pm[:, 0:512], st[:, 0:512], AF.Exp, r=[st_b], w=[pm_b], scale=0.125)

    def dC(k):
        idx, r, p_, j, g, d, nbn = geom(k)
        i = idx % 2
        pm, pm_b, nd, nd_b = Pm[k % 3], Pm_b[k % 3], ndp[k % 3], ndp_b[k % 3]
        for b in range(2):
            nb = 2 * p_ + b
            nt_ = 2 if nb > 0 else 1
            o = b * 256
            for t in range(nt_):
                tidx = r * nbn + nb - t
                mm(P, nd[:, b * 128:(b + 1) * 128], Vr[i][:, tidx, :], pm[:, o + t * 128:o + (t + 1) * 128], t == 0,
                   t == nt_ - 1, r=[hd_b[i], pm_b], w=[nd_b])
            for t in range(nt_):
                mm(P, nd[:, 256 + b * 128:256 + (b + 1) * 128], onb[:, :], pm[:, o + t * 128:o + (t + 1) * 128],
                   t == 0, t == nt_ - 1, r=[mb_b, pm_b], w=[nd_b])

    def dD(k):
        idx, r, p_, j, g, d, nbn = geom(k)
        nd, nd_b = ndp[k % 3], ndp_b[k % 3]
        ND, ND_b = NDs[j % 2], NDs_b[j % 2]
        base = 2 * p_ * 128 * d + r
        dst = ND[:, :, qsl(base, d, 256)]
        src = nd[:, :].rearrange("p (a q) -> p a q", a=2)
        if g == 0:
            cp(P, "dve", dst, src, r=[nd_b], w=[ND_b])
        else:
            tt(P, "dve", dst, src, dst, ALU.add, r=[nd_b, ND_b], w=[ND_b])
        if k == last_of[idx] and g == 2:
            act(P, ND[:, 1, :], ND[:, 1, :], AF.Ln, r=[ND_b], w=[ND_b])
            act(P, ND[:, 1, :], ND[:, 1, :], AF.Exp, r=[ND_b], w=[ND_b], scale=-1.0)
            tt(P, "pool", OA[:], ND[:, 0, :], ND[:, 1, :], ALU.mult, r=[ND_b], w=[OA_b])
            P.dma("sp", C.OAT[j * 64:(j + 1) * 64, :], OA[:], owner=OA_b, r=[OA_b], w=[C.OAT_b])

    load_head(0)
    for step in range(NBLK + 3):
        for lag, fn in ((3, dD), (2, dC), (1, dB), (0, dA)):
            k = step - lag
            if 0 <= k < NBLK:
                fn(k)
        if step < NBLK:
            idx = blocks[step][0]
            if step == min(first_of[idx] + 3, last_of[idx]) and idx + 1 < len(heads):
                load_head(idx + 1)
    P.barrier()
    es.close()
    P.release(bufs)


class Ring:
    def __init__(self, P, tiles, name):
        self.t = tiles
        self.b = [P.buf("%s%d" % (name, i)) for i in range(len(tiles))]
        self.i = 0

    def next(self):
        k = self.i % len(self.t)
        self.i += 1
        return self.t[k], self.b[k]


def phase_C(P, C, li):
    nc = P.nc
    es = contextlib.ExitStack()
    sb = lambda name, shape, dt: es.enter_context(nc.sbuf_tensor(name + "_L%d" % li, shape, dt))
    ps = lambda name, shape, dt: es.enter_context(nc.psum_tensor(name + "_L%d" % li, shape, dt))
    bufs = []

    def B(name, dma=False):
        b = P.buf(name, dma)
        bufs.append(b)
        return b

    Wua = sb("C_wua", [128, 2, D], BF16); Wua_b = B("C_wua", True)
    Wub = sb("C_wub", [128, 4, D], BF16); Wub_b = B("C_wub", True)
    Wuc = sb("C_wuc", [128, 4, D], BF16); Wuc_b = B("C_wuc", True)
    Wg = sb("C_wg", [128, NKC, 3 * D], BF16); Wg_b = B("C_wg", True)
    Wo = sb("C_wo", [128, NKC, D], BF16); Wo_b = B("C_wo", True)
    bg = sb("C_bg", [128, 24], F32); bg_b = B("C_bg", True)
    hT = [sb("C_hT%d" % i, [128, NKC, TT], BF16) for i in range(2)]
    oa = [sb("C_oa%d" % i, [128, 2, TT], BF16) for i in range(2)]
    ob = [sb("C_ob%d" % i, [128, 4, TT], BF16) for i in range(2)]
    yc = [sb("C_yc%d" % i, [128, 4, TT], BF16) for i in range(2)]
    in_b = [B("C_in%d" % i, True) for i in range(2)]
    xt = [sb("C_xt%d" % i, [128, 4, D], F32) for i in range(2)]
    xt_b = [B("C_xt%d" % i, True) for i in range(2)]
    mT = [sb("C_mT%d" % i, [128, NKC, TT], BF16) for i in range(2)]
    mT_b = [B("C_mT%d" % i) for i in range(2)]
    gs = [sb("C_gs%d" % i, [128, TT], F32) for i in range(3)]
    gs_b = [B("C_gs%d" % i) for i in range(3)]
    macc = [sb("C_ma%d" % i, [128, TT], F32) for i in range(2)]
    macc_b = [B("C_ma%d" % i) for i in range(2)]
    tmp = [sb("C_tm%d" % i, [128, TT], F32) for i in range(2)]
    tmp_b = [B("C_tm%d" % i) for i in range(2)]
    ring = Ring(P, [ps("C_ps%d" % i, [128, 512], F32) for i in range(8)], "C_ps")

    P.dma("pool", Wua[:], C.w_up_a[li].rearrange("(kc p) d -> p kc d", p=128), owner=Wua_b, w=[Wua_b])
    P.dma("pool", Wub[:], C.w_up_b[li].rearrange("(kc p) d -> p kc d", p=128), owner=Wub_b, w=[Wub_b])
    P.dma("pool", Wuc[:], C.w_up_c[li].rearrange("(kc p) d -> p kc d", p=128), owner=Wuc_b, w=[Wuc_b])
    for kc in range(NKC):
        P.dma("pool", Wg[:, kc, :], C.w_gate[li][kc * 128:(kc + 1) * 128, :], owner=Wg_b, w=[Wg_b])
    for kc in range(NKC):
        P.dma("pool", Wo[:, kc, :], C.w_out[li][kc * 128:(kc + 1) * 128, :], owner=Wo_b, w=[Wo_b])
    P.dma("sp", bg[:], C.b_gate[li].rearrange("(c p) -> p c", p=128), owner=bg_b, w=[bg_b], slow=True)

    xd = C.x_in if li == 0 else C.x_dram
    fm = lambda ap: ap.rearrange("(kc p) t -> p kc t", p=128)

    def load(ti):
        pb = ti % 2
        t0 = ti * TT
        P.dma("sp", hT[pb][:], fm(C.HT[:, t0:t0 + TT]), owner=in_b[pb], r=[C.HT_b], w=[in_b[pb]])
        P.dma("sp", oa[pb][:], fm(C.OAT[:, t0:t0 + TT]), owner=in_b[pb], r=[C.OAT_b], w=[in_b[pb]])
        P.dma("sp", ob[pb][:], fm(C.OBT[:, t0:t0 + TT]), owner=in_b[pb], r=[C.OBT_b], w=[in_b[pb]])
        P.dma("sp", yc[pb][:], fm(C.YCT[:, t0:t0 + TT]), owner=in_b[pb], r=[C.YCT_b], w=[in_b[pb]])
        P.dma("sp", xt[pb][:], xd[t0:t0 + TT, :].rearrange("(b p) d -> p b d", p=128), owner=xt_b[pb],
              r=[C.xd_b[ti]], w=[xt_b[pb]])

    load(0)
    gi = 0
    ai = 0
    for ti in range(NTT):
        pb = ti % 2
        t0 = ti * TT
        if ti + 1 < NTT:
            load(ti + 1)
        for fc in range(NKC):
            ma = macc[ai % 2]
            ma_b = macc_b[ai % 2]
            ai += 1
            for j, (Wu, Wu_b, nk, src) in enumerate(((Wua, Wua_b, 2, oa), (Wub, Wub_b, 4, ob), (Wuc, Wuc_b, 4, yc))):
                py, py_b = ring.next()
                for kc in range(nk):
                    mm(P, py[:], Wu[:, kc, fc * 128:(fc + 1) * 128], src[pb][:, kc, :], kc == 0, kc == nk - 1,
                       r=[Wu_b, in_b[pb]], w=[py_b])
                pg, pg_b = ring.next()
                c0 = j * D + fc * 128
                for kc in range(NKC):
                    mm(P, pg[:], Wg[:, kc, c0:c0 + 128], hT[pb][:, kc, :], kc == 0, kc == NKC - 1,
                       r=[Wg_b, in_b[pb]], w=[pg_b])
                g = gs[gi % 3]
                g_b = gs_b[gi % 3]
                gi += 1
                act(P, g[:], pg[:], AF.Sigmoid, r=[pg_b, bg_b], w=[g_b], bias=bg[:, j * 8 + fc:j * 8 + fc + 1])
                if j == 0:
                    tt(P, "dve", ma[:], py[:], g[:], ALU.mult, r=[py_b, g_b], w=[ma_b])
                else:
                    tm = tmp[j % 2]
                    tm_b = tmp_b[j % 2]
                    tt(P, "dve", tm[:], py[:], g[:], ALU.mult, r=[py_b, g_b], w=[tm_b])
                    if j == 1:
                        tt(P, "pool", ma[:], ma[:], tm[:], ALU.add, r=[ma_b, tm_b], w=[ma_b])
                    else:
                        tt(P, "pool", mT[pb][:, fc, :], ma[:], tm[:], ALU.add, r=[ma_b, tm_b], w=[mT_b[pb]])
        for bi in range(4):
            for hf in range(2):
                po, po_b = ring.next()
                for kc in range(NKC):
                    mm(P, po[:], mT[pb][:, kc, bi * 128:(bi + 1) * 128], Wo[:, kc, hf * 512:(hf + 1) * 512], kc == 0,
                       kc == NKC - 1, r=[mT_b[pb], Wo_b], w=[po_b])
                xs_ = xt[pb][:, bi, hf * 512:(hf + 1) * 512]
                tt(P, "dve", xs_, po[:], xs_, ALU.add, r=[po_b, xt_b[pb]], w=[xt_b[pb]])
        P.dma("sp", C.x1_dram[t0:t0 + TT, :].rearrange("(b p) d -> p b d", p=128), xt[pb][:], owner=xt_b[pb],
              r=[xt_b[pb]], w=[C.x1d_b])
    P.barrier()
    es.close()
    P.release(bufs)


def phase_D(P, C, li):
    nc = P.nc
    es = contextlib.ExitStack()
    sb = lambda name, shape, dt: es.enter_context(nc.sbuf_tensor(name + "_L%d" % li, shape, dt))
    ps = lambda name, shape, dt: es.enter_context(nc.psum_tensor(name + "_L%d" % li, shape, dt))
    bufs = []

    def B(name, dma=False):
        b = P.buf(name, dma)
        bufs.append(b)
        return b

    last = (li == DEPTH - 1)
    NB = TS // 128
    NT5 = TS // 512
    yacc = sb("D_y", [128, NB, D], F32)
    y_b = [B("D_y%d" % i) for i in range(NB)]
    yld_b = [B("D_yld%d" % i, True) for i in range(4)]
    hmT = sb("D_hmT", [128, NKC, TS], BF16)
    hm_b = [B("D_hm%d" % i) for i in range(NB)]
    hfT = [sb("D_hfT%d" % i, [128, NKC, 128], F32) for i in range(2)]
    hf_b = [B("D_hf%d" % i) for i in range(2)]
    NW = 2
    Weg = [sb("D_weg%d" % i, [128, NKC, EH], BF16) for i in range(NW)]
    Weu = [sb("D_weu%d" % i, [128, NKC, EH], BF16) for i in range(NW)]
    Wed = [sb("D_wed%d" % i, [128, 2, D], BF16) for i in range(NW)]
    We_b = [B("D_we%d" % i, True) for i in range(NW)]
    Wr = sb("D_wr", [128, NKC, 20], F32); Wr_b = B("D_wr", True)
    br = sb("D_br", [128, 20], F32); br_b = B("D_br", True)
    Wpg = sb("D_wpg", [128, NKC, D], BF16); Wpg_b = B("D_wpg", True)
    Wpi = sb("D_wpi", [128, 2, D], BF16); Wpi_b = B("D_wpi", True)
    gbT2 = sb("D_gbT2", [128, NKC, 128], F32); gb2_b = B("D_gb2")
    gate = sb("D_gate", [128, NB, 16], F32)
    gate_b = [B("D_gate%d" % i) for i in range(NB)]
    rt = sb("D_rt", [128, 64], F32); rt_b = B("D_rt")
    junk = sb("D_junk", [128, D], BF16); junk_b = B("D_junk")
    ss = sb("D_ss", [128, 4], F32); ss_b = B("D_ss")
    ssr = [sb("D_ssr%d" % i, [128, 4], F32) for i in range(3)]
    ssr_b = [B("D_ssr%d" % i) for i in range(3)]
    xs = [sb("D_xs%d" % i, [128, D], F32) for i in range(2)]
    xs_b = [B("D_xs%d" % i) for i in range(2)]
    hid = [sb("D_hid%d" % i, [128, 2, 512], BF16) for i in range(2)]
    hid_b = [[B("D_hid%d_%d" % (i, f)) for f in range(2)] for i in range(2)]
    sl = [sb("D_sl%d" % i, [128, 512], F32) for i in range(4)]
    sl_b = [B("D_sl%d" % i) for i in range(4)]
    hpT = [sb("D_hpT%d" % i, [128, NKC, 128], BF16) for i in range(2)]
    hp_b = [B("D_hp%d" % i) for i in range(2)]
    pin = [sb("D_pin%d" % i, [128, PLE], F32) for i in range(2)]
    pin_b = [B("D_pin%d" % i, True) for i in range(2)]
    pT = [sb("D_pT%d" % i, [128, 2, 128], BF16) for i in range(2)]
    pT_b = [B("D_pT%d" % i) for i in range(2)]
    sg = [sb("D_sg%d" % i, [128, 512], F32) for i in range(2)]
    sg_b = [B("D_sg%d" % i) for i in range(2)]
    junk2 = sb("D_junk2", [128, D], BF16); junk2_b = B("D_junk2")
    ss2 = sb("D_ss2", [128, 4], F32); ss2_b = B("D_ss2")
    st_b = [B("D_st%d" % i, True) for i in range(4)]
    if last:
        gfin = sb("D_gfin", [128, D], F32); gfin_b = B("D_gfin", True)
        ob_ = [sb("D_ob0", [128, D], F32)] * 2
        ob_b = [B("D_ob0", True)] * 2
    ring = Ring(P, [ps("D_ps%d" % i, [128, 512], F32) for i in range(8)], "D_ps")

    P.dma("sp", Wr[:, :, 0:4], C.w_rg[li].rearrange("(kc p) g -> p kc g", p=128), owner=Wr_b, w=[Wr_b], slow=True)
    for g in range(4):
        P.dma("sp", Wr[:, :, 4 + 4 * g:8 + 4 * g], C.w_re[li][g].rearrange("(kc p) e -> p kc e", p=128), owner=Wr_b,
              w=[Wr_b], slow=True)
    P.dma("sp", br[:, 0:4], C.b_rg[li].partition_broadcast(128), owner=br_b, w=[br_b])
    P.dma("sp", br[:, 4:20], C.b_re[li].partition_broadcast(128), owner=br_b, w=[br_b])
    for kc in range(NKC):
        P.dma("pool", Wpg[:, kc, :], C.w_pg[li][kc * 128:(kc + 1) * 128, :], owner=Wpg_b, w=[Wpg_b])
    P.dma("pool", Wpi[:], C.w_pi[li].rearrange("(kc p) d -> p kc d", p=128), owner=Wpi_b, w=[Wpi_b])
    load_gain(P, C, C.norm_moe[li], C.gbT, "D")
    load_gain(P, C, C.norm_ple[li], gbT2, "D2", gb_b=gb2_b)
    if last:
        P.dma("sp", gfin[:], C.norm_final.partition_broadcast(128), owner=gfin_b, w=[gfin_b])

    def load_expert(k):
        e = k % NEXP
        i = k % NW
        P.dma("pool", Weg[i][:], C.w_eg[li][e].rearrange("(kc p) f -> p kc f", p=128), owner=We_b[i], w=[We_b[i]])
        P.dma("pool", Weu[i][:], C.w_eu[li][e].rearrange("(kc p) f -> p kc f", p=128), owner=We_b[i], w=[We_b[i]])
        P.dma("pool", Wed[i][:], C.w_ed[li][e].rearrange("(fc p) d -> p fc d", p=128), owner=We_b[i], w=[We_b[i]])

    NST = S // TS
    ek = 0
    hi_ = 0
    blk_g = 0
    for sti in range(NST):
        T0 = sti * TS
        for q in range(NB // 4):
            qq = q % 4
            P.dma("sp", yacc[:, q * 4:(q + 1) * 4, :],
                  C.x1_dram[T0 + q * 512:T0 + (q + 1) * 512, :].rearrange("(b p) d -> p b d", p=128),
                  owner=yld_b[qq], r=[C.x1d_b], w=[y_b[q * 4 + j] for j in range(4)])
        if sti == 0:
            load_expert(ek)
        nstate = {}

        def n0(blk):
            sq = ssr[blk % 3]
            sq_b = ssr_b[blk % 3]
            act(P, junk[:], yacc[:, blk, :], AF.Square, r=[y_b[blk]], w=[junk_b, sq_b], accum_out=sq[:, 0:1])
            act(P, sq[:, 1:2], sq[:, 0:1], AF.Ln, r=[sq_b, C.cst_b], w=[sq_b], scale=1.0 / D, bias=C.eps_col[:, 0:1])
            act(P, sq[:, 2:3], sq[:, 1:2], AF.Exp, r=[sq_b], w=[sq_b], scale=-0.5)

        def n1(blk):
            sq = ssr[blk % 3]
            sq_b = ssr_b[blk % 3]
            x_ = xs[blk % 2]
            x_b = xs_b[blk % 2]
            ts(P, "dve", x_[:], yacc[:, blk, :], sq[:, 2:3], ALU.mult, r=[y_b[blk], sq_b], w=[x_b])
            tp = [ring.next(), ring.next()]
            nstate[blk] = tp
            for h in range(2):
                for j in range(4):
                    kc = h * 4 + j
                    tr(P, tp[h][0][:, j * 128:(j + 1) * 128], x_[:, kc * 128:(kc + 1) * 128], C.ident[:],
                       r=[x_b, C.ident_b], w=[tp[h][1]])

        def n2(blk):
            tp = nstate.pop(blk)
            f = hfT[blk % 2]
            f_b = hf_b[blk % 2]
            for h in range(2):
                src = tp[h][0][:].rearrange("p (j t) -> p j t", j=4)
                tt(P, "dve", hmT[:, h * 4:(h + 1) * 4, blk * 128:(blk + 1) * 128], src,
                   C.gbT[:, h * 4:(h + 1) * 4, :], ALU.mult, r=[tp[h][1], C.gb_b], w=[hm_b[blk]])
                tt(P, "dve", f[:, h * 4:(h + 1) * 4, :], src, C.gbT[:, h * 4:(h + 1) * 4, :], ALU.mult,
                   r=[tp[h][1], C.gb_b], w=[f_b])

        def router_mm(blk):
            f = hfT[blk % 2]
            f_b = hf_b[blk % 2]
            pr, pr_b = ring.next()
            nstate[("r", blk)] = (pr, pr_b)
            for kc in range(NKC):
                mm(P, pr[:, 0:20], f[:, kc, :], Wr[:, kc, :], kc == 0, kc == NKC - 1, r=[f_b, Wr_b], w=[pr_b])

        def router(blk):
            pr, pr_b = nstate.pop(("r", blk))
            L = rt[:, 0:20]
            tt(P, "dve", L, pr[:, 0:20], br[:], ALU.add, r=[pr_b, br_b], w=[rt_b])
            m = rt[:, 20:21]
            negm = rt[:, 21:22]
            oh = rt[:, 24:28]
            ex = rt[:, 28:32]
            se = rt[:, 22:23]
            p1 = rt[:, 23:24]
            sel = rt[:, 32:36]
            mk1 = rt[:, 36:40]
            s2 = rt[:, 40:44]
            mk2 = rt[:, 44:48]
            m1 = rt[:, 48:49]
            m2 = rt[:, 49:50]
            dd_ = rt[:, 50:51]
            ed = rt[:, 51:52]
            e1 = rt[:, 52:53]
            a1 = rt[:, 53:54]
            a2 = rt[:, 54:55]
            g4 = rt[:, 56:60]
            R_ = dict(r=[rt_b], w=[rt_b])
            P.op("dve", lambda: nc.vector.tensor_reduce(out=m, in_=rt[:, 0:4], axis=AX.X, op=ALU.max), **R_)
            ts(P, "dve", oh, rt[:, 0:4], m, ALU.is_ge, **R_)
            ts(P, "dve", negm, m, -1.0, ALU.mult, **R_)
            act(P, ex, rt[:, 0:4], AF.Exp, r=[rt_b], w=[rt_b], bias=negm, accum_out=se)
            P.op("dve", lambda: nc.vector.reciprocal(out=p1, in_=se), **R_)
            ts(P, "dve", sel, rt[:, 4:8], oh[:, 0:1], ALU.mult, **R_)
            for g in range(1, 4):
                stt(P, "dve", sel, rt[:, 4 + 4 * g:8 + 4 * g], oh[:, g:g + 1], sel, ALU.mult, ALU.add, **R_)
            P.op("dve", lambda: nc.vector.tensor_reduce(out=m1, in_=sel, axis=AX.X, op=ALU.max), **R_)
            ts(P, "dve", mk1, sel, m1, ALU.is_ge, **R_)
            stt(P, "dve", s2, mk1, -1e30, sel, ALU.mult, ALU.add, **R_)
            P.op("dve", lambda: nc.vector.tensor_reduce(out=m2, in_=s2, axis=AX.X, op=ALU.max), **R_)
            ts(P, "dve", mk2, s2, m2, ALU.is_ge, **R_)
            tt(P, "dve", dd_, m2, m1, ALU.subtract, **R_)
            act(P, ed, dd_, AF.Exp, r=[rt_b], w=[rt_b])
            ts(P, "dve", e1, ed, 1.0, ALU.add, **R_)
            P.op("dve", lambda: nc.vector.reciprocal(out=e1, in_=e1), **R_)
            tt(P, "dve", a1, e1, p1, ALU.mult, **R_)
            tt(P, "dve", a2, ed, a1, ALU.mult, **R_)
            ts(P, "dve", g4, mk1, a1, ALU.mult, **R_)
            stt(P, "dve", g4, mk2, a2, g4, ALU.mult, ALU.add, **R_)
            for g in range(4):
                ts(P, "dve", gate[:, blk, 4 * g:4 * g + 4], g4, oh[:, g:g + 1], ALU.mult, r=[rt_b], w=[gate_b[blk]])

        units = [(e, t5) for e in range(NEXP) for t5 in range(NT5)]
        wslot = {}

        def gu(u):
            nonlocal ek
            e, t5 = units[u]
            if t5 == 0:
                wslot[e] = ek % NW
                ek += 1
            i = wslot[e]
            hd = hid[u % 2]
            hd_b = hid_b[u % 2]
            for fch in range(2):
                pg, pg_b = ring.next()
                for kc in range(NKC):
                    mm(P, pg[:], Weg[i][:, kc, fch * 128:(fch + 1) * 128], hmT[:, kc, t5 * 512:(t5 + 1) * 512],
                       kc == 0, kc == NKC - 1, r=[We_b[i]] + hm_b[t5 * 4:t5 * 4 + 4], w=[pg_b])
                pu, pu_b = ring.next()
                for kc in range(NKC):
                    mm(P, pu[:], Weu[i][:, kc, fch * 128:(fch + 1) * 128], hmT[:, kc, t5 * 512:(t5 + 1) * 512],
                       kc == 0, kc == NKC - 1, r=[We_b[i]] + hm_b[t5 * 4:t5 * 4 + 4], w=[pu_b])
                s_ = sl[(u % 2) * 2 + fch]
                s_b = sl_b[(u % 2) * 2 + fch]
                act(P, s_[:], pg[:], AF.Silu, r=[pg_b], w=[s_b])
                tt(P, "dve", hd[:, fch, :], pu[:], s_[:], ALU.mult, r=[pu_b, s_b], w=[hd_b[fch]])

        def down(u):
            e, t5 = units[u]
            i = wslot[e]
            hd = hid[u % 2]
            hd_b = hid_b[u % 2]
            for half in range(2):
                grp = []
                for bi in (2 * half, 2 * half + 1):
                    for hf in range(2):
                        po, po_b = ring.next()
                        grp.append((bi, hf, po, po_b))
                for fch in range(2):
                    for (bi, hf, po, po_b) in grp:
                        mm(P, po[:], hd[:, fch, bi * 128:(bi + 1) * 128], Wed[i][:, fch, hf * 512:(hf + 1) * 512],
                           fch == 0, fch == 1, r=[hd_b[fch], We_b[i]], w=[po_b])
                for (bi, hf, po, po_b) in grp:
                    blk = t5 * 4 + bi
                    ya = yacc[:, blk, hf * 512:(hf + 1) * 512]
                    stt(P, "dve", ya, po[:], gate[:, blk, e:e + 1], ya, ALU.mult, ALU.add,
                        r=[po_b, gate_b[blk], y_b[blk]], w=[y_b[blk]])

        def unit(u):
            gu(u)
            down(u)
            if units[u][1] == 0 and not (sti == NST - 1 and units[u][0] == NEXP - 1):
                load_expert(ek)

        n0(0)
        for step in range(NB + 2):
            if step + 1 < NB:
                n0(step + 1)
            if step >= 1 and step - 1 < NB:
                router_mm(step - 1)
            if step < NB:
                n1(step)
            if step >= 1 and step - 1 < NB:
                router(step - 1)
            if step < NB:
                n2(step)
            if step >= 4 and step % 4 == 0 and step // 4 - 1 < NT5:
                unit(step // 4 - 1)
        for u in range(NT5, len(units)):
            unit(u)

        def ple0(blk):
            tok = T0 + blk * 128
            i2 = blk % 2
            P.dma("sp", pin[i2][:], C.p_in[li][tok:tok + 128, :], owner=pin_b[i2], w=[pin_b[i2]])

        def ple1(blk):
            i2 = blk % 2
            t0, t0_b = ring.next()
            t1, t1_b = ring.next()
            norm_T(P, C, yacc[:, blk, :], y_b[blk], gbT2, hpT[i2], hp_b[i2], 0, junk, junk_b, ss, ss_b,
                   xs[i2], xs_b[i2], [t0, t1], [t0_b, t1_b], gb_b=gb2_b)
            pp, pp_b = ring.next()
            for c in range(2):
                tr(P, pp[:, c * 128:(c + 1) * 128], pin[i2][:, c * 128:(c + 1) * 128], C.ident[:],
                   r=[pin_b[i2], C.ident_b], w=[pp_b])
            cp(P, "act", pT[i2][:], pp[:, 0:256].rearrange("p (c t) -> p c t", c=2), r=[pp_b], w=[pT_b[i2]])

        def ple2(blk):
            i2 = blk % 2
            for hf in range(2):
                p1_, p1_b = ring.next()
                for kc in range(NKC):
                    mm(P, p1_[:], hpT[i2][:, kc, :], Wpg[:, kc, hf * 512:(hf + 1) * 512], kc == 0, kc == NKC - 1,
                       r=[hp_b[i2], Wpg_b], w=[p1_b])
                p2_, p2_b = ring.next()
                for c in range(2):
                    mm(P, p2_[:], pT[i2][:, c, :], Wpi[:, c, hf * 512:(hf + 1) * 512], c == 0, c == 1,
                       r=[pT_b[i2], Wpi_b], w=[p2_b])
                sg_ = sg[hf]
                sgb = sg_b[hf]
                act(P, sg_[:], p1_[:], AF.Sigmoid, r=[p1_b], w=[sgb])
                tt(P, "dve", sg_[:], p2_[:], sg_[:], ALU.mult, r=[p2_b, sgb], w=[sgb])
                ya = yacc[:, blk, hf * 512:(hf + 1) * 512]
                tt(P, "pool", ya, ya, sg_[:], ALU.add, r=[sgb, y_b[blk]], w=[y_b[blk]])

        def ple3(blk):
            tok = T0 + blk * 128
            i2 = blk % 2
            if not last:
                P.dma("sp", C.x_dram[tok:tok + 128, :], yacc[:, blk, :], owner=st_b[blk % 4], r=[y_b[blk]],
                      w=[C.xd_b[0]])
            else:
                o_ = ob_[i2]
                o_b = ob_b[i2]
                act(P, junk2[:], yacc[:, blk, :], AF.Square, r=[y_b[blk]], w=[junk2_b, ss2_b], accum_out=ss2[:, 0:1])
                act(P, ss2[:, 1:2], ss2[:, 0:1], AF.Ln, r=[ss2_b, C.cst_b], w=[ss2_b], scale=1.0 / D,
                    bias=C.eps_col[:, 0:1])
                act(P, ss2[:, 2:3], ss2[:, 1:2], AF.Exp, r=[ss2_b], w=[ss2_b], scale=-0.5)
                stt(P, "dve", o_[:], yacc[:, blk, :], ss2[:, 2:3], gfin[:], ALU.mult, ALU.mult,
                    r=[y_b[blk], ss2_b, gfin_b], w=[o_b])
                P.dma("sp", C.out[tok:tok + 128, :], o_[:], owner=o_b, r=[o_b], w=[C.out_b])

        PLE_LAGS = ((0, ple0), (0, ple1), (0, ple2), (0, ple3)) if not PLE_PIPE else \
            ((2, ple3), (1, ple2), (0, ple1), (-1, ple0))
        for step in range(-1, NB + 2):
            for lag, fn in PLE_LAGS:
                b_ = step - lag
                if 0 <= b_ < NB:
                    fn(b_)
        P.barrier()
    es.close()
    P.release(bufs)


def build_program(stages=("A", "Bs", "Bd", "C", "D"), debug=(), nlayers=DEPTH):
    nc = bass.Bass("TRN2", target_bir_lowering=False)
    P = Prog(nc)
    C = Ctx()
    ext = lambda name, shape, dt: nc.dram_tensor(name, shape, dt, kind="ExternalInput").ap()
    C.x_in = ext("x", [S, D], F32)
    C.p_in = ext("p", [DEPTH, S, PLE], F32)
    C.positions = ext("positions", [S], I32)
    C.norm_mix = ext("norm_mix", [DEPTH, D], F32)
    C.w_in = ext("w_in", [DEPTH, D, INW], F32)
    C.w_gate = ext("w_gate", [DEPTH, D, 3 * D], F32)
    C.b_gate = ext("b_gate", [DEPTH, 3 * D], F32)
    C.w_pool = ext("w_pool", [DEPTH, 4, 128, 128], F32)
    C.pool_scale = ext("pool_scale", [DEPTH, 512], F32)
    C.w_up_a = ext("w_up_a", [DEPTH, 256, D], F32)
    C.w_up_b = ext("w_up_b", [DEPTH, 512, D], F32)
    C.w_up_c = ext("w_up_c", [DEPTH, 512, D], F32)
    C.w_out = ext("w_out", [DEPTH, D, D], F32)
    C.norm_moe = ext("norm_moe", [DEPTH, D], F32)
    C.w_rg = ext("w_router_grp", [DEPTH, D, 4], F32)
    C.b_rg = ext("b_router_grp", [DEPTH, 4], F32)
    C.w_re = ext("w_router_exp", [DEPTH, 4, D, 4], F32)
    C.b_re = ext("b_router_exp", [DEPTH, 16], F32)
    C.w_eg = ext("w_exp_gate", [DEPTH, NEXP, D, EH], F32)
    C.w_eu = ext("w_exp_up", [DEPTH, NEXP, D, EH], F32)
    C.w_ed = ext("w_exp_down", [DEPTH, NEXP, EH, D], F32)
    C.norm_ple = ext("norm_ple", [DEPTH, D], F32)
    C.w_pi = ext("w_ple_in", [DEPTH, PLE, D], F32)
    C.w_pg = ext("w_ple_gate", [DEPTH, D, D], F32)
    C.norm_final = ext("norm_final", [D], F32)
    C.ident_in = ext("c_ident", [128, 128], F32)
    C.invf_in = ext("c_invf", [128, 8], F32)
    C.rcnt_in = ext("c_rcnt", [4, 16], F32)
    C.tri_in = ext("c_tri", [128, 128], F32)
    C.mband_in = ext("c_mband", [128, 256], F32)
    C.out = nc.dram_tensor("out", [S, D], F32, kind="ExternalOutput").ap()
    C.out_b = P.buf("out")

    dram = lambda name, shape, dt: nc.dram_tensor(name, shape, dt).ap()
    C.x_dram = dram("x_scr", [S, D], F32)
    C.xd_b = [P.buf("xd%d" % i) for i in range(NTT)]
    C.x_dram_b = C.xd_b[0]
    C.QTd = dram("QTd", [DILW, S], BF16); C.QTd_b = P.buf("QTd")
    C.KTd = dram("KTd", [DILW, S], BF16); C.KTd_b = P.buf("KTd")
    C.Vd = dram("Vd", [S, DILW], BF16); C.Vd_b = P.buf("Vd")
    C.QTs = dram("QTs", [SBW, S], BF16); C.QTs_b = P.buf("QTs")
    C.KTs = dram("KTs", [SBW, S], BF16); C.KTs_b = P.buf("KTs")
    C.Vs = dram("Vs", [S, SBW], BF16); C.Vs_b = P.buf("Vs")
    C.YCT = dram("YCT", [512, S], BF16); C.YCT_b = P.buf("YCT")
    C.OAT = dram("OAT", [256, S], BF16); C.OAT_b = P.buf("OAT")
    C.OBT = dram("OBT", [512, S], BF16); C.OBT_b = P.buf("OBT")
    C.HT = dram("HT", [D, S], BF16); C.HT_b = P.buf("HT")
    C.x1_dram = dram("x1_scr", [S, D], F32); C.x1d_b = P.buf("x1d"); C.x1_dram_b = C.x1d_b

    es = contextlib.ExitStack()
    sb = lambda name, shape, dt: es.enter_context(nc.sbuf_tensor(name, shape, dt))
    C.ident = sb("c_ident_sb", [128, 128], F32); C.ident_b = P.buf("ident", True)
    C.invf = sb("c_invf_sb", [128, 8], F32)
    C.sgn = C.invf[:, 1:2]
    C.pi_col = C.invf[:, 2:3]
    C.eps_col = C.invf[:, 3:4]
    C.one_col = C.invf[:, 4:5]
    C.cst_b = P.buf("cst", True)
    C.ones_f = sb("c_ones_f", [128, 512], F32); C.ones_b = P.buf("ones")
    C.gcol = sb("c_gcol", [128, 8], F32); C.gcol_b = P.buf("gcol", True)
    C.gbT = sb("c_gbT", [128, NKC, 128], F32); C.gb_b = P.buf("gbT")
    C.rcnt16 = sb("c_rc16", [128, 4, 16], F32)
    C.rcnt_b = P.buf("rcnt", True)

    P.dma("sp", C.ident[:], C.ident_in[:, :], owner=C.ident_b, w=[C.ident_b])
    P.dma("sp", C.invf[:], C.invf_in[:, :], owner=C.cst_b, w=[C.cst_b])
    P.dma("sp", C.rcnt16[:], C.rcnt_in.partition_broadcast(128), owner=C.rcnt_b, w=[C.rcnt_b])
    P.op("dve", lambda: nc.vector.memset(C.ones_f[:], 1.0), w=[C.ones_b])
    for li in range(DEPTH):
        if "A" in stages:
            phase_A(P, C, li)
        if "Bs" in stages:
            phase_B_sb(P, C, li)
        if "Bd" in stages:
            phase_B_dil(P, C, li)
        if "C" in stages:
            phase_C(P, C, li)
        if "D" in stages:
            phase_D(P, C, li)
        if li + 1 >= nlayers:
            break

    dbg_b = P.buf("dbg", True)
    for name in debug:
        src = getattr(C, name)
        o = nc.dram_tensor("dbg_" + name, list(src.shape), src.dtype, kind="ExternalOutput").ap()
        P.dma("sp", o, src, owner=dbg_b, r=[getattr(C, name + "_b")], w=[dbg_b])
    P.barrier()
    es.close()
    return nc, P


def host_consts():
    ident = np.eye(128, dtype=np.float32)
    invf = np.zeros((128, 8), np.float32)
    p = np.arange(128)
    half = 32
    invf[:, 0] = (10000.0 ** (-(p % 32).astype(np.float32) / np.float32(half))).astype(np.float32)
    invf[:, 1] = np.where((p % 64) < 32, -1.0, 1.0)
    invf[:, 2] = np.pi
    invf[:, 3] = EPS
    invf[:, 4] = 1.0
    t = np.arange(16)
    rcnt = np.stack([1.0 / np.minimum(t + 1, w) for w in POOLW]).astype(np.float32)
    tri = (np.arange(128)[None, :] < np.arange(128)[:, None]).astype(np.float32)
    kk = np.arange(128)[:, None]
    qq = np.arange(128)[None, :]
    mband = np.concatenate([(kk <= qq), (kk >= qq)], axis=1).astype(np.float32)
    return {"c_ident": ident, "c_invf": invf, "c_rcnt": rcnt, "c_tri": tri, "c_mband": mband}


def make_in_maps(inputs):
    consts = host_consts()
    maps = []
    for c in range(8):
        b = c % 4
        m = dict(consts)
        m["x"] = np.ascontiguousarray(inputs["x"][b])
        m["p"] = np.ascontiguousarray(inputs["p"][:, b])
        m["positions"] = np.ascontiguousarray(inputs["positions"][b]).astype(np.int32)
        for k in ("norm_mix", "w_in", "w_gate", "b_gate", "w_pool", "pool_scale", "w_up_a", "w_up_b", "w_up_c",
                  "w_out", "norm_moe", "w_router_grp", "b_router_grp", "w_router_exp", "w_exp_gate", "w_exp_up",
                  "w_exp_down", "norm_ple", "w_ple_in", "w_ple_gate", "norm_final"):
            m[k] = np.ascontiguousarray(inputs[k], dtype=np.float32)
        m["b_router_exp"] = np.ascontiguousarray(inputs["b_router_exp"], dtype=np.float32).reshape(DEPTH, 16)
        maps.append(m)
    return maps


def kernel(**inputs):
    inputs = {k: np.asarray(v) for k, v in inputs.items()}
    nc, _ = build_program()
    res = run_bass_kernel_spmd(nc, make_in_maps(inputs), core_ids=list(range(8)))
    out = np.stack([res.results[b]["out"] for b in range(4)], axis=0)
    return out.astype(np.float32)
```
